# Optimizing a Trainium2 kernel written in Bass

```python
import math
import jax, jax.numpy as jnp
from jax import lax
import numpy as np

D_MODEL = 1024
BATCH = 8
SEQ = 4096
DEPTH = 2

HEAD_DIM = 64
N_HEADS_MOBA = 6
N_HEADS_MLA = 4
N_HEADS_FOX = 6
MOBA_BLOCK = 256
MOBA_TOPK = 3
MOBA_Q_BLOCK = 32
Q_BLOCK = 128
MLA_Q_RANK = 256
MLA_KV_RANK = 128
MLA_NOPE_DIM = 64
MLA_ROPE_DIM = 32
MLA_V_DIM = 64
MLA_QK_DIM = MLA_NOPE_DIM + MLA_ROPE_DIM
ROPE_THETA = 10000.0
T5_BUCKETS = 32
T5_MAX_DIST = 1024
D_FF = 2816
CONV_WIDTH = 3
NORM_EPS = 1e-6
NEG_INF = -1e30

W_MOBA = N_HEADS_MOBA * HEAD_DIM
W_MLA = N_HEADS_MLA * MLA_V_DIM
W_FOX = N_HEADS_FOX * HEAD_DIM
MIX_WIDTH = W_MOBA + W_MLA + W_FOX
PROJ_SIZES = (W_MOBA, W_MOBA, W_MOBA, MLA_Q_RANK, MLA_KV_RANK, MLA_ROPE_DIM, W_FOX, W_FOX, W_FOX, N_HEADS_FOX)
PROJ_COLS = sum(PROJ_SIZES)

kernel_name = "hymba_moba_mla_fox_convffn"


def rms_norm(x, g):
    xf = x.astype(jnp.float32)
    y = xf * lax.rsqrt(jnp.mean(xf * xf, axis=-1, keepdims=True) + NORM_EPS)
    return (y * g.astype(jnp.float32)).astype(x.dtype)


def split_heads(t, n, d):
    b, s, _ = t.shape
    return t.reshape(b, s, n, d).transpose(0, 2, 1, 3)


def merge_heads(t):
    b, h, s, d = t.shape
    return t.transpose(0, 2, 1, 3).reshape(b, s, h * d)


def rotary(t, pos):
    half = t.shape[-1] // 2
    inv = ROPE_THETA ** (-jnp.arange(half, dtype=jnp.float32) / half)
    ang = pos.astype(jnp.float32)[:, None] * inv[None, :]
    cos, sin = jnp.cos(ang), jnp.sin(ang)
    t1 = t[..., :half].astype(jnp.float32)
    t2 = t[..., half:].astype(jnp.float32)
    return jnp.concatenate([t1 * cos - t2 * sin, t1 * sin + t2 * cos], axis=-1).astype(t.dtype)


def t5_bucket(rel):
    n = jnp.maximum(rel, 0)
    exact = T5_BUCKETS // 2
    large = exact + (jnp.log(jnp.maximum(n, 1).astype(jnp.float32) / exact)
                     / math.log(T5_MAX_DIST / exact) * (T5_BUCKETS - exact)).astype(jnp.int32)
    return jnp.where(n < exact, n, jnp.minimum(large, T5_BUCKETS - 1))


def moba_attention(q, k, v, rel_bias):
    b, h, s, dh = q.shape
    nb = -(-s // MOBA_BLOCK)
    pad = nb * MOBA_BLOCK - s
    kp = jnp.pad(k, ((0, 0), (0, 0), (0, pad), (0, 0)))
    vp = jnp.pad(v, ((0, 0), (0, 0), (0, pad), (0, 0)))
    k_blocks = kp.reshape(b, h, nb, MOBA_BLOCK, dh)
    v_blocks = vp.reshape(b, h, nb, MOBA_BLOCK, dh)
    k_mean = jnp.mean(k_blocks, axis=3)
    topk = min(MOBA_TOPK, nb)
    bias_tab = rel_bias.T
    scale = dh ** -0.5
    b_idx = jnp.arange(b)[:, None, None, None]
    h_idx = jnp.arange(h)[None, :, None, None]
    blk_pos = jnp.arange(MOBA_BLOCK)

    def chunk(c):
        q_start = c * MOBA_Q_BLOCK
        qc = lax.dynamic_slice_in_dim(q, q_start, MOBA_Q_BLOCK, axis=2)
        q_pos = q_start + jnp.arange(MOBA_Q_BLOCK)
        own = q_start // MOBA_BLOCK
        gate = jnp.einsum('bhqd,bhnd->bhqn', qc, k_mean).astype(jnp.float32)
        gate = jnp.where(jnp.arange(nb) < own, gate, NEG_INF)
        _, sel = lax.top_k(gate, topk)
        valid = jnp.arange(topk) < own
        k_sel = k_blocks[b_idx, h_idx, sel]
        v_sel = v_blocks[b_idx, h_idx, sel]
        k_pos_sel = sel[..., None] * MOBA_BLOCK + blk_pos
        s_sel = jnp.einsum('bhqd,bhqnkd->bhqnk', qc, k_sel).astype(jnp.float32) * scale
        s_sel = s_sel + bias_tab[h_idx[..., None], t5_bucket(q_pos[:, None, None] - k_pos_sel)]
        s_sel = jnp.where(valid[:, None], s_sel, NEG_INF)
        s_sel = s_sel.reshape(b, h, MOBA_Q_BLOCK, topk * MOBA_BLOCK)
        own_start = own * MOBA_BLOCK
        k_own = lax.dynamic_slice_in_dim(kp, own_start, MOBA_BLOCK, axis=2)
        v_own = lax.dynamic_slice_in_dim(vp, own_start, MOBA_BLOCK, axis=2)
        rel = q_pos[:, None] - (own_start + blk_pos)[None, :]
        s_own = jnp.einsum('bhqd,bhkd->bhqk', qc, k_own).astype(jnp.float32) * scale
        s_own = jnp.where(rel >= 0, s_own + bias_tab[:, t5_bucket(rel)], NEG_INF)
        p = jax.nn.softmax(jnp.concatenate([s_sel, s_own], axis=-1), axis=-1).astype(v.dtype)
        p_sel = p[..., :topk * MOBA_BLOCK].reshape(b, h, MOBA_Q_BLOCK, topk, MOBA_BLOCK)
        p_own = p[..., topk * MOBA_BLOCK:]
        return (jnp.einsum('bhqnk,bhqnkd->bhqd', p_sel, v_sel)
                + jnp.einsum('bhqk,bhkd->bhqd', p_own, v_own))

    outs = lax.map(chunk, jnp.arange(s // MOBA_Q_BLOCK))
    return outs.transpose(1, 2, 0, 3, 4).reshape(b, h, s, dh)


def causal_block_attention(q, k, v, scale, decay=None):
    b, h, s, _ = q.shape
    dv = v.shape[-1]
    k_pos = jnp.arange(s)

    def chunk(c):
        q_start = c * Q_BLOCK
        qc = lax.dynamic_slice_in_dim(q, q_start, Q_BLOCK, axis=2)
        q_pos = q_start + jnp.arange(Q_BLOCK)
        sc = jnp.einsum('bhqd,bhkd->bhqk', qc, k).astype(jnp.float32) * scale
        if decay is not None:
            dq = lax.dynamic_slice_in_dim(decay, q_start, Q_BLOCK, axis=2)
            sc = sc + dq[..., None] - decay[:, :, None, :]
        sc = jnp.where(k_pos[None, :] <= q_pos[:, None], sc, NEG_INF)
        p = jax.nn.softmax(sc, axis=-1).astype(v.dtype)
        return jnp.einsum('bhqk,bhkd->bhqd', p, v)

    outs = lax.map(chunk, jnp.arange(s // Q_BLOCK))
    return outs.transpose(1, 2, 0, 3, 4).reshape(b, h, s, dv)


def hybrid_mixer(h, rel_bias, w_in, b_f, q_norm, kv_norm, w_uq, w_ukv, w_o):
    b, s, _ = h.shape
    pos = jnp.arange(s)
    proj = h @ w_in
    (a_q, a_k, a_v, c_q, c_kv, k_r, f_q, f_k, f_v, f_g) = jnp.split(
        proj, np.cumsum(PROJ_SIZES)[:-1].tolist(), axis=-1)

    out_a = moba_attention(split_heads(a_q, N_HEADS_MOBA, HEAD_DIM),
                           split_heads(a_k, N_HEADS_MOBA, HEAD_DIM),
                           split_heads(a_v, N_HEADS_MOBA, HEAD_DIM), rel_bias)

    qb = split_heads(rms_norm(c_q, q_norm) @ w_uq, N_HEADS_MLA, MLA_QK_DIM)
    kv = split_heads(rms_norm(c_kv, kv_norm) @ w_ukv, N_HEADS_MLA, MLA_NOPE_DIM + MLA_V_DIM)
    k_nope, v_b = kv[..., :MLA_NOPE_DIM], kv[..., MLA_NOPE_DIM:]
    q_b = jnp.concatenate([qb[..., :MLA_NOPE_DIM], rotary(qb[..., MLA_NOPE_DIM:], pos)], axis=-1)
    k_rope = jnp.broadcast_to(rotary(k_r[:, None], pos), (b, N_HEADS_MLA, s, MLA_ROPE_DIM))
    k_b = jnp.concatenate([k_nope, k_rope], axis=-1)
    out_b = causal_block_attention(q_b, k_b, v_b, MLA_QK_DIM ** -0.5)

    log_f = jax.nn.log_sigmoid(f_g.astype(jnp.float32) + b_f.astype(jnp.float32))
    decay = jnp.cumsum(log_f, axis=1).transpose(0, 2, 1)
    out_c = causal_block_attention(split_heads(f_q, N_HEADS_FOX, HEAD_DIM),
                                   split_heads(f_k, N_HEADS_FOX, HEAD_DIM),
                                   split_heads(f_v, N_HEADS_FOX, HEAD_DIM),
                                   HEAD_DIM ** -0.5, decay)

    merged = jnp.concatenate([merge_heads(out_a), merge_heads(out_b), merge_heads(out_c)], axis=-1)
    return merged @ w_o


def conv_ffn(h, w_up, conv_w, conv_b, w_down):
    u = h @ w_up
    u = lax.conv_general_dilated(u, conv_w[:, None, :], window_strides=(1,),
                                 padding=[(CONV_WIDTH - 1, 0)],
                                 dimension_numbers=('NWC', 'WIO', 'NWC'),
                                 feature_group_count=u.shape[-1]) + conv_b
    g, val = jnp.split(u, 2, axis=-1)
    return (jax.nn.gelu(g, approximate=True) * val) @ w_down


def setup_inputs(seed: int = 0) -> dict:
    key = jax.random.key(seed)
    ks = jax.random.split(key, 20)
    f32 = jnp.float32

    def nrm(k, shape, fan_in):
        return jax.random.normal(k, shape, f32) * (fan_in ** -0.5)

    def gain(k, shape):
        return 1.0 + 0.1 * jax.random.normal(k, shape, f32)

    L = DEPTH
    return {
        "x": jax.random.normal(ks[0], (BATCH, SEQ, D_MODEL), f32),
        "rel_bias": 0.5 * jax.random.normal(ks[1], (T5_BUCKETS, N_HEADS_MOBA), f32),
        "ln_mix_pre": gain(ks[2], (L, D_MODEL)),
        "ln_mix_post": gain(ks[3], (L, D_MODEL)),
        "ln_ffn_pre": gain(ks[4], (L, D_MODEL)),
        "ln_ffn_post": gain(ks[5], (L, D_MODEL)),
        "w_in": nrm(ks[6], (L, D_MODEL, PROJ_COLS), D_MODEL),
        "b_f": 2.0 + 0.5 * jax.random.normal(ks[7], (L, N_HEADS_FOX), f32),
        "q_norm": gain(ks[8], (L, MLA_Q_RANK)),
        "kv_norm": gain(ks[9], (L, MLA_KV_RANK)),
        "w_uq": nrm(ks[10], (L, MLA_Q_RANK, N_HEADS_MLA * MLA_QK_DIM), MLA_Q_RANK),
        "w_ukv": nrm(ks[11], (L, MLA_KV_RANK, N_HEADS_MLA * (MLA_NOPE_DIM + MLA_V_DIM)), MLA_KV_RANK),
        "w_o": nrm(ks[12], (L, MIX_WIDTH, D_MODEL), MIX_WIDTH),
        "w_up": nrm(ks[13], (L, D_MODEL, 2 * D_FF), D_MODEL),
        "conv_w": nrm(ks[14], (L, CONV_WIDTH, 2 * D_FF), CONV_WIDTH),
        "conv_b": 0.01 * jax.random.normal(ks[15], (L, 2 * D_FF), f32),
        "w_down": nrm(ks[16], (L, D_FF, D_MODEL), D_FF),
    }


def reference(x, rel_bias, ln_mix_pre, ln_mix_post, ln_ffn_pre, ln_ffn_post, w_in, b_f,
              q_norm, kv_norm, w_uq, w_ukv, w_o, w_up, conv_w, conv_b, w_down):
    for l in range(DEPTH):
        h = rms_norm(x, ln_mix_pre[l])
        x = x + rms_norm(hybrid_mixer(h, rel_bias, w_in[l], b_f[l], q_norm[l], kv_norm[l],
                                      w_uq[l], w_ukv[l], w_o[l]), ln_mix_post[l])
        h = rms_norm(x, ln_ffn_pre[l])
        x = x + rms_norm(conv_ffn(h, w_up[l], conv_w[l], conv_b[l], w_down[l]), ln_ffn_post[l])
    return x
```

```python
import math
import os
from contextlib import ExitStack
import numpy as np
import concourse.bass as bass
import concourse.mybir as mybir
from concourse.bass_utils import run_bass_kernel_spmd

F32 = mybir.dt.float32
BF16 = mybir.dt.bfloat16
AF = mybir.ActivationFunctionType
ALU = mybir.AluOpType
AX = mybir.AxisListType

D = 1024
DFF = 2816
NCH_UP = 44
BIG = 30000.0
EPS = 1e-6
WIN = 2888
VOFF = 2120
GW = 1920
SCALE_A = 64 ** -0.5
SCALE_B = 96 ** -0.5
WARM = False


class Res:
    __slots__ = ("name", "w", "r", "pw", "pr", "excl")

    def __init__(self, name="", excl=False):
        self.name = name
        self.excl = excl
        self.w = {}
        self.r = {}
        self.pw = {}
        self.pr = {}


class FW:
    NDMA = 8

    def __init__(self, nc, stack):
        self.nc = nc
        self.eng = ("pe", "dve", "act", "pool", "sp")
        self.sems = {}
        self.cnt = {}
        for e in ("pe", "dve", "act", "pool"):
            self.sems[e] = stack.enter_context(nc.semaphore("s_" + e))
            self.cnt[e] = 0
        self.drr = {}
        for q in ("sp", "pool"):
            self.drr[q] = 0
            for i in range(self.NDMA):
                k = "d_%s%d" % (q, i)
                self.sems[k] = stack.enter_context(nc.semaphore(k))
                self.cnt[k] = 0
        self.seen = {e: {} for e in self.eng}
        self.streams = {e: [] for e in self.eng}
        self.nops = 0
        self.nflush = 0
        self.stop = None
        self.dead = False
        self.killed = False
        self.stop_stage = None

    def stage(self, name):
        if name == self.stop_stage:
            self.dead = True

    def _wait(self, e, deps):
        seen = self.seen[e]
        for k, v in deps.items():
            if e == "pe" and k == "pe":
                continue
            if seen.get(k, 0) < v:
                self.streams[e].append(("w", self.sems[k], v))
                seen[k] = v

    def _deps(self, reads, writes, wadd):
        deps = {}

        def add(k, v):
            if deps.get(k, 0) < v:
                deps[k] = v
        for t in reads:
            for k, v in t.w.items():
                add(k, v)
            if t.excl:
                for k, v in t.r.items():
                    add(k, v)
        for t in writes:
            for k, v in t.w.items():
                add(k, v)
            for k, v in t.r.items():
                add(k, v)
        for t in wadd:
            for k, v in t.r.items():
                add(k, v)
            for k, v in t.pw.items():
                add(k, v)
            for k, v in t.pr.items():
                add(k, v)
        return deps

    def _mark(self, k, v, reads, writes, wadd):
        for t in reads:
            if t.r.get(k, 0) < v:
                t.r[k] = v
        for t in writes:
            t.pw = t.w
            t.pr = t.r
            t.w = {k: v}
            t.r = {}
        for t in wadd:
            t.w[k] = v

    def op(self, e, fn, reads=(), writes=(), wadd=()):
        if self.dead:
            return
        self._wait(e, self._deps(reads, writes, wadd))
        self.streams[e].append(("i", fn, self.sems[e], 1))
        self.cnt[e] += 1
        self._mark(e, self.cnt[e], reads, writes, wadd)
        self.nops += 1

    def dma(self, q, out, in_, reads=(), writes=(), wadd=()):
        if self.dead:
            return
        i = self.drr[q]
        self.drr[q] = (i + 1) % self.NDMA
        k = "d_%s%d" % (q, i)
        deps = self._deps(reads, writes, wadd)
        if self.cnt[k] > 0 and deps.get(k, 0) < self.cnt[k]:
            deps[k] = self.cnt[k]
        self._wait(q, deps)
        self.streams[q].append(("i", (lambda e, out=out, in_=in_: e.dma_start(out=out, in_=in_)), self.sems[k], 16))
        self.cnt[k] += 16
        self._mark(k, self.cnt[k], reads, writes, wadd)
        self.nops += 1

    def flush(self):
        if self.killed:
            return
        self.nflush += 1
        if self.dead or (self.stop is not None and self.nflush >= self.stop):
            self.dead = True
            self.killed = True
        deps = {k: v for k, v in self.cnt.items() if v > 0}
        for e in self.eng:
            self._wait(e, deps)
        streams = self.streams

        def rp(lst):
            def f(eng):
                for it in lst:
                    if it[0] == "w":
                        eng.wait_ge(it[1], it[2])
                    else:
                        it[1](eng).then_inc(it[2], it[3])
            return f
        with self.nc.Block() as block:
            block.sync(rp(streams["sp"]))
            block.tensor(rp(streams["pe"]))
            block.vector(rp(streams["dve"]))
            block.scalar(rp(streams["act"]))
            block.gpsimd(rp(streams["pool"]))
        self.streams = {e: [] for e in self.eng}


def MM(out, lhsT, rhs, start=True, stop=True):
    return lambda e: e.matmul(out, lhsT=lhsT, rhs=rhs, start=start, stop=stop)


def TR(out, in_, ident):
    return lambda e: e.transpose(out=out, in_=in_, identity=ident)


def ACT(out, in_, func, **kw):
    return lambda e: e.activation(out=out, in_=in_, func=func, **kw)


def TS(out, in0, s1, s2, op0, op1=None):
    if op1 is None:
        return lambda e: e.tensor_scalar(out=out, in0=in0, scalar1=s1, scalar2=None, op0=op0)
    return lambda e: e.tensor_scalar(out=out, in0=in0, scalar1=s1, scalar2=s2, op0=op0, op1=op1)


def STT(out, in0, scalar, in1, op0, op1):
    return lambda e: e.scalar_tensor_tensor(out=out, in0=in0, scalar=scalar, in1=in1, op0=op0, op1=op1)


def TT(out, in0, in1, op):
    return lambda e: e.tensor_tensor(out=out, in0=in0, in1=in1, op=op)


def CP(out, in_):
    return lambda e: e.tensor_copy(out=out, in_=in_)


def MS(ap, val):
    return lambda e: e.memset(ap, val)


def build(S, L, dbg=False, stop=None, stop_stage=None):
    NT = S // 128
    NG = S // 512
    NBLK = S // 256
    nc = bass.Bass("TRN2", target_bir_lowering=False)

    def dram(name, shape, dtype, kind="Internal"):
        return nc.dram_tensor(name, shape, dtype, kind=kind).ap()
    EI = "ExternalInput"
    SCR = "ExternalOutput" if dbg else "Internal"
    x_in = dram("x", [S, D], F32, EI)
    out = dram("out", [S, D], F32, "ExternalOutput")
    w_in = dram("w_in", [L, D, WIN], F32, EI)
    g_mixpre = dram("g_mixpre", [L, 128, 8], F32, EI)
    g_ffnpre = dram("g_ffnpre", [L, 128, 8], F32, EI)
    g_mixpost = dram("g_mixpost", [L, 128, D], F32, EI)
    g_ffnpost = dram("g_ffnpost", [L, 128, D], F32, EI)
    b_f = dram("b_f", [6, L], F32, EI)
    qn = dram("qn", [L, 128, 2], F32, EI)
    kvn = dram("kvn", [L, 128, 1], F32, EI)
    w_uq = dram("w_uq", [L, 256, 768], F32, EI)
    w_ukvk = dram("w_ukvk", [L, 128, 256], F32, EI)
    w_ukvv = dram("w_ukvv", [L, 128, 256], F32, EI)
    w_o = dram("w_o", [L, D, D], F32, EI)
    w_up = dram("w_up", [L, D, 2 * DFF], F32, EI)
    convp = dram("convp", [L, 128, NCH_UP, 4], F32, EI)
    w_down = dram("w_down", [L, DFF, D], F32, EI)
    Gd = dram("G", [128, 6, GW], F32, EI)
    trid = dram("tri", [128, 128], F32, EI)
    identd = dram("ident", [128, 128], F32, EI)
    cosd = dram("cosT", [128, S], F32, EI)
    sind = dram("sinS", [128, S], F32, EI)
    blkd = dram("blk1h", [16, S], F32, EI)

    QT = dram("QT", [16, 128, S], BF16, SCR)
    KT = dram("KT", [16, 128, S], BF16, SCR)
    VA = dram("VA", [S, 16 * 66], BF16, SCR)
    MT = dram("MT", [D, S], BF16, SCR)
    X1 = dram("X1", [S, D], F32, SCR)
    XM = dram("XM", [S, D], F32, SCR)

    R_QT = [Res("QT%d" % i) for i in range(16)]
    R_KT = [Res("KT%d" % i) for i in range(16)]
    R_VA = Res("VA")
    R_MT = Res("MT")
    R_X1 = Res("X1")
    R_XM = Res("XM")
    R_OUT = Res("out")
    R_IN = Res("in")

    with ExitStack() as top:
        fw = FW(nc, top)
        fw.stop = stop
        fw.stop_stage = stop_stage

        uniq = [0]

        def sbuf(st, name, shape, dtype):
            uniq[0] += 1
            return st.enter_context(nc.sbuf_tensor("%s_%d" % (name, uniq[0]), shape, dtype))

        PS = [top.enter_context(nc.psum_tensor("ps%d" % i, [128, 512], F32)) for i in range(7)]
        R_PS = [Res("ps%d" % i, excl=True) for i in range(7)]
        PSB = top.enter_context(nc.psum_tensor("psb", [128, 1024], BF16))
        R_PSB = Res("psb", excl=True)
        psrr = [0]

        def getps(lo=0, hi=7):
            i = psrr[0]
            psrr[0] = (i + 1 - lo) % (hi - lo) + lo
            return PS[i], R_PS[i]

        identb = sbuf(top, "identb", [128, 128], BF16)
        trib = sbuf(top, "trib", [128, 128], BF16)
        onesb = sbuf(top, "onesb", [128, 128], BF16)
        onesf = sbuf(top, "onesf", [128, 512], F32)
        mhalf = sbuf(top, "mhalf", [128, 512], F32)
        selrow = sbuf(top, "selrow", [128, 64], F32)
        epsb = sbuf(top, "epsb", [128, 1], F32)
        negbf = sbuf(top, "negbf", [6, L], F32)
        R_C = Res("consts")
        fw.dma("pool", identb[:], identd[:], writes=[R_C])
        fw.dma("pool", trib[:], trid[:], wadd=[R_C])
        fw.op("pool", MS(onesb[:], 1.0), wadd=[R_C])
        fw.op("pool", MS(onesf[:], 1.0), wadd=[R_C])
        fw.op("pool", MS(mhalf[:], -0.5), wadd=[R_C])
        fw.op("dve", MS(selrow[:], 0.0), wadd=[R_C])
        fw.op("dve", MS(epsb[:], EPS), wadd=[R_C])
        fw.op("dve", MS(selrow[64:65, :], 1.0), reads=[R_C], wadd=[R_C])
        fw.dma("sp", negbf[:], b_f[:], wadd=[R_C])
        fw.op("dve", TS(negbf[:], negbf[:], -1.0, None, ALU.mult), reads=[R_C], wadd=[R_C])
        with ExitStack() as ph:
            c1h = sbuf(ph, "c1h", [16, S], BF16)
            c1 = sbuf(ph, "c1", [3, S], BF16)
            R1 = Res()
            fw.dma("pool", c1h[:].rearrange("p (a n) -> p a n", n=512), blkd.rearrange("p (a n) -> p a n", n=512), writes=[R1])
            fw.op("dve", MS(c1[:], 1.0), wadd=[R1])
            for h in range(6):
                fw.dma("sp", KT[h, 64:80, :], c1h[:], reads=[R1], wadd=[R_KT[h]])
                fw.dma("sp", KT[6 + h, 64:67, :], c1[:], reads=[R1], wadd=[R_KT[6 + h]])
                fw.dma("sp", QT[6 + h, 67:70, :], c1[:], reads=[R1], wadd=[R_QT[6 + h]])
            fw.flush()

        def load_scaled_weight(st_tiles, R_st, ctr, dst, src, gain, Rdst):
            n = src.shape[-1]
            c0 = 0
            while c0 < n:
                c1_ = min(n, c0 + 1408)
                b = ctr[0] % 2
                ctr[0] += 1
                fw.dma("sp", st_tiles[b][:, 0:c1_ - c0], src[:, c0:c1_], writes=[R_st[b]])
                fw.op("dve", TS(dst[:, c0:c1_], st_tiles[b][:, 0:c1_ - c0], gain, None, ALU.mult),
                      reads=[R_st[b], R_C], wadd=[Rdst])
                c0 = c1_

        def rstd_act(dst, src_ps, Rsrc, Rdst, n):
            p = dst.shape[0]
            fw.op("act", ACT(dst, src_ps, AF.Ln, scale=1.0 / n, bias=epsb[0:p, 0:1]), reads=[Rsrc, R_C], writes=[Rdst])
            fw.op("act", ACT(dst, dst, AF.Exp, scale=-0.5), reads=[Rdst], writes=[Rdst])

        def rms_rstd(ss_ap, rstd_ap, Rs, n, eng2="pool"):
            fw.op("dve", TS(ss_ap, ss_ap, 1.0 / n, EPS, ALU.mult, ALU.add), reads=[Rs], writes=[Rs])
            p = ss_ap.shape[0]
            f = ss_ap.shape[1]
            fw.op("pool", TT(rstd_ap, ss_ap, mhalf[0:p, 0:f], ALU.pow), reads=[Rs, R_C], writes=[Rs])

        def norm_part(xt, Rx, junk, Rj, stat, Rstat, hb, Rhb):
            fw.op("act", ACT(junk[:], xt[:], AF.Square, accum_out=stat[:, 0:1]), reads=[Rx], writes=[Rj, Rstat])
            rms_rstd(stat[:, 0:1], stat[:, 1:2], Rstat, D)
            fw.op("act", ACT(hb[:], xt[:], AF.Copy, scale=stat[:, 1:2]), reads=[Rx, Rstat], writes=[Rhb])

        def norm_transpose(xt, Rx, junk, Rj, stat, Rstat, hb, Rhb, hT, RhT, col0, first):
            norm_part(xt, Rx, junk, Rj, stat, Rstat, hb, Rhb)
            tr_part(hb, Rhb, hT, RhT, col0, first)

        def tr_part(hb, Rhb, hT, RhT, col0, first):
            for k in range(8):
                fw.op("pe", TR(PSB[:, k * 128:(k + 1) * 128], hb[:, k * 128:(k + 1) * 128], identb[:]),
                      reads=[Rhb, R_C], writes=[R_PSB] if k == 0 else [], wadd=[R_PSB] if k else [])
            kw = dict(writes=[RhT]) if first else dict(wadd=[RhT])
            fw.op("dve", CP(hT[:, :, col0:col0 + 128], PSB[:].rearrange("p (k n) -> p k n", k=8)),
                  reads=[R_PSB], **kw)

        for l in range(L):
            xsrc, Rxsrc = (x_in, R_IN) if l == 0 else (XM, R_XM)
            xdst, Rxdst = (out, R_OUT) if l == L - 1 else (XM, R_XM)

            with ExitStack() as ph:
                winb = sbuf(ph, "winb", [128, 8, WIN], BF16)
                R_W = Res("winb")
                wuqb = sbuf(ph, "wuqb", [128, 2, 768], BF16)
                wkb = sbuf(ph, "wkb", [128, 256], BF16)
                wvb = sbuf(ph, "wvb", [128, 256], BF16)
                gpre = sbuf(ph, "gpre", [128, 8], F32)
                qng = sbuf(ph, "qng", [128, 2], F32)
                kvng = sbuf(ph, "kvng", [128, 1], F32)
                wst = [sbuf(ph, "wst%d" % i, [128, 1408], F32) for i in range(2)]
                R_wst = [Res() for _ in range(2)]
                xin = [sbuf(ph, "xin%d" % i, [128, D], F32) for i in range(2)]
                R_xin = [Res() for _ in range(2)]
                junk = sbuf(ph, "junk", [128, D], BF16)
                R_junk = Res()
                stat = [sbuf(ph, "stat%d" % i, [128, 8], F32) for i in range(2)]
                R_stat = [Res() for _ in range(2)]
                hb = [sbuf(ph, "hb%d" % i, [128, D], BF16) for i in range(2)]
                R_hb = [Res() for _ in range(2)]
                hT = [sbuf(ph, "hT%d" % i, [128, 8, 512], BF16) for i in range(2)]
                R_hT = [Res() for _ in range(2)]
                stg = [sbuf(ph, "stg%d" % i, [128, 512], BF16) for i in range(3)]
                R_stg = [Res() for _ in range(3)]
                kmT = sbuf(ph, "kmT", [128, 3, 2, 16], BF16)
                R_km = Res()
                gsb = sbuf(ph, "gsb", [128, 4, 6, 16], F32)
                R_gsb = Res()
                mx8 = sbuf(ph, "mx8", [128, 4, 6, 8], F32)
                R_mx = Res()
                penf = sbuf(ph, "penf", [128, 4, 6, 16], F32)
                penb = sbuf(ph, "penb", [128, 4, 96], BF16)
                R_pen = Res()
                penT = sbuf(ph, "penT", [128, 512], BF16)
                R_penT = Res()
                cqn = sbuf(ph, "cqn", [128, 2, 512], BF16)
                sqq = sbuf(ph, "sqq", [128, 2, 512], BF16)
                R_cq = Res()
                ckvn = sbuf(ph, "ckvn", [128, 512], BF16)
                sqk = sbuf(ph, "sqk", [128, 512], BF16)
                R_ckv = Res()
                rq = sbuf(ph, "rq", [128, 512], F32)
                R_rq = Res()
                rk = sbuf(ph, "rk", [128, 512], F32)
                R_rk = Res()
                rkt = sbuf(ph, "rkt", [128, 8], F32)
                R_rkt = Res()
                cosb = sbuf(ph, "cosb", [128, 512], F32)
                sinb = sbuf(ph, "sinb", [128, 512], F32)
                R_cs = Res()
                qa = sbuf(ph, "qa", [128, 512], F32)
                qr = sbuf(ph, "qr", [128, 512], F32)
                R_qa = Res()
                vst = [sbuf(ph, "vst%d" % i, [128, 16, 66], BF16) for i in range(2)]
                R_vst = [Res() for _ in range(2)]
                fe = sbuf(ph, "fe", [6, 512], F32)
                fsc = sbuf(ph, "fsc", [6, 512], F32)
                fr = sbuf(ph, "fr", [6, 512], F32)
                fcar = sbuf(ph, "fcar", [6, 1], F32)
                fsp = sbuf(ph, "fsp", [6, 3, 512], BF16)
                fsn = sbuf(ph, "fsn", [6, 3, 512], BF16)
                R_f = Res()
                R_fs = Res()

                fw.dma("sp", gpre[:], g_mixpre[l], writes=[R_C])
                fw.dma("sp", qng[:], qn[l], wadd=[R_C])
                fw.dma("sp", kvng[:], kvn[l], wadd=[R_C])
                ctr = [0]
                for k in range(8):
                    load_scaled_weight(wst, R_wst, ctr, winb[:, k, :], w_in[l, k * 128:(k + 1) * 128, :],
                                       gpre[:, k:k + 1], R_W)
                fw.dma("pool", wuqb[:], w_uq[l].rearrange("(k p) n -> p k n", p=128), wadd=[R_W])
                fw.dma("pool", wkb[:], w_ukvk[l], wadd=[R_W])
                fw.dma("pool", wvb[:], w_ukvv[l], wadd=[R_W])
                fw.op("pool", MS(kmT[:], 0.0), writes=[R_km])
                for b in range(2):
                    fw.op("pool", MS(vst[b][:, :, 64:65], 1.0), writes=[R_vst[b]])
                    fw.op("pool", MS(vst[b][:, :, 65:66], 0.0), wadd=[R_vst[b]])
                fw.op("pool", MS(fcar[:], 0.0), writes=[R_f])

                def load_x(ti):
                    b = ti % 2
                    fw.dma("sp", xin[b][:], xsrc[ti * 128:(ti + 1) * 128, :], reads=[Rxsrc], writes=[R_xin[b]])

                def fchunk(hTg, RhTg, c0, m):
                    p, Rp = getps()
                    for k in range(8):
                        fw.op("pe", MM(p[0:m, :], winb[:, k, c0:c0 + m], hTg[:, k, :], start=(k == 0), stop=(k == 7)),
                              reads=[R_W, RhTg], writes=[Rp] if k == 0 else [], wadd=[Rp] if k else [])
                    return p, Rp

                stgrr = [0]

                def getstg():
                    i = stgrr[0]
                    stgrr[0] = (i + 1) % 3
                    return stg[i], R_stg[i]

                def front_norm(ti):
                    if ti + 1 < NT:
                        load_x(ti + 1)
                    b = ti % 2
                    norm_part(xin[b], R_xin[b], junk, R_junk, stat[b], R_stat[b], hb[b], R_hb[b])

                def front_tr(ti):
                    b = ti % 2
                    gg, i = ti // 4, ti % 4
                    tr_part(hb[b], R_hb[b], hT[gg % 2], R_hT[gg % 2], i * 128, i == 0)

                def proj(g):
                    t0 = g * 512
                    hTg, RhTg = hT[g % 2], R_hT[g % 2]
                    fw.stage("A")
                    fw.dma("sp", cosb[64:96, :], cosd[64:96, t0:t0 + 512], writes=[R_cs])
                    fw.dma("sp", sinb[64:96, :], sind[64:96, t0:t0 + 512], wadd=[R_cs])
                    for c in (3, 4, 5, 9, 10, 11):
                        p, Rp = fchunk(hTg, RhTg, c * 128, 128)
                        s_, Rs_ = getstg()
                        if c < 6:
                            for j in range(2):
                                fw.op("act", ACT(s_[:, j * 256:(j + 1) * 256], p[:, j * 256:(j + 1) * 256], AF.Copy,
                                                 accum_out=qa[:, j:j + 1]), reads=[Rp],
                                      **(dict(writes=[Rs_, R_qa]) if j == 0 else dict(wadd=[Rs_, R_qa])))
                            for hh in range(2):
                                fw.op("act", ACT(kmT[hh * 64:(hh + 1) * 64, c - 3, hh, 2 * g:2 * g + 2], qa[hh * 64:(hh + 1) * 64, 0:2],
                                                 AF.Copy, scale=1.0 / 256), reads=[R_qa], wadd=[R_km])
                        else:
                            fw.op("act", ACT(s_[:], p[:], AF.Copy), reads=[Rp], writes=[Rs_])
                        hd0 = 2 * (c - 3) if c < 6 else 6 + 2 * (c - 9)
                        for hh in range(2):
                            fw.dma("sp", KT[hd0 + hh, 0:64, t0:t0 + 512], s_[hh * 64:(hh + 1) * 64, :],
                                   reads=[Rs_], wadd=[R_KT[hd0 + hh]])
                    yield
                    fw.stage("B")
                    pg, Rpg = getps()
                    for c in (0, 1, 2):
                        p, Rp = fchunk(hTg, RhTg, c * 128, 128)
                        s_, Rs_ = getstg()
                        fw.op("act", ACT(s_[:], p[:], AF.Copy, scale=SCALE_A), reads=[Rp], writes=[Rs_])
                        for hh in range(2):
                            fw.dma("sp", QT[2 * c + hh, 0:64, t0:t0 + 512], s_[hh * 64:(hh + 1) * 64, :],
                                   reads=[Rs_], wadd=[R_QT[2 * c + hh]])
                        for i in range(4):
                            o = (i * 6 + 2 * c) * 16
                            first = (c == 0 and i == 0)
                            fw.op("pe", MM(pg[:, o:o + 32], s_[:, i * 128:(i + 1) * 128],
                                           kmT[:, c, :, :].rearrange("p a n -> p (a n)")),
                                  reads=[Rs_, R_km], writes=[Rpg] if first else [], wadd=[] if first else [Rpg])
                    fw.op("dve", CP(gsb[:].rearrange("p a b c -> p (a b c)"), pg[:, 0:384]), reads=[Rpg], writes=[R_gsb])
                    for i in range(4):
                        own = (g * 4 + i) // 2
                        fw.op("pool", MS(gsb[:, i, :, own:16], -BIG), reads=[R_gsb], wadd=[R_gsb])
                    for i in range(4):
                        for h in range(6):
                            fw.op("dve", lambda e, i=i, h=h: e.max(out=mx8[:, i, h, :], in_=gsb[:, i, h, :]),
                                  reads=[R_gsb], **(dict(writes=[R_mx]) if (i == 0 and h == 0) else dict(wadd=[R_mx])))
                    for i in range(4):
                        for h in range(6):
                            fw.op("dve", TS(penf[:, i, h, :], gsb[:, i, h, :], mx8[:, i, h, 2:3], BIG, ALU.is_ge, ALU.mult),
                                  reads=[R_gsb, R_mx], wadd=[R_pen])
                    fw.op("dve", TS(penb[:].rearrange("p a b -> p (a b)"), penf[:].rearrange("p a b c -> p (a b c)"),
                                    -BIG, None, ALU.add), reads=[R_pen], writes=[R_pen])
                    for i in range(4):
                        own = (g * 4 + i) // 2
                        fw.op("dve", MS(penb[:, i, :].rearrange("p (h n) -> p h n", h=6)[:, :, own:own + 1], 0.0),
                              reads=[R_pen], wadd=[R_pen])
                    for i in range(4):
                        fw.op("pe", TR(PSB[0:96, i * 128:(i + 1) * 128], penb[:, i, :], identb[:]),
                              reads=[R_pen, R_C], writes=[R_PSB] if i == 0 else [], wadd=[R_PSB] if i else [])
                    fw.op("dve", CP(penT[0:96, :], PSB[0:96, 0:512]), reads=[R_PSB], writes=[R_penT])
                    for h in range(6):
                        fw.dma("sp", QT[h, 64:80, t0:t0 + 512], penT[h * 16:(h + 1) * 16, :], reads=[R_penT],
                               wadd=[R_QT[h]])
                    fw.stage("C")
                    for c in (6, 7, 8):
                        p, Rp = fchunk(hTg, RhTg, c * 128, 128)
                        s_, Rs_ = getstg()
                        fw.op("act", ACT(s_[:], p[:], AF.Copy, scale=SCALE_A), reads=[Rp], writes=[Rs_])
                        for hh in range(2):
                            hd = 6 + 2 * (c - 6) + hh
                            fw.dma("sp", QT[hd, 0:64, t0:t0 + 512], s_[hh * 64:(hh + 1) * 64, :], reads=[Rs_],
                                   wadd=[R_QT[hd]])
                    yield
                    fw.stage("D")
                    for cc in range(2):
                        p, Rp = fchunk(hTg, RhTg, (12 + cc) * 128, 128)
                        kw = dict(writes=[R_cq]) if cc == 0 else dict(wadd=[R_cq])
                        fw.op("dve", TS(cqn[:, cc, :], p[:], qng[:, cc:cc + 1], None, ALU.mult), reads=[Rp, R_C], **kw)
                        fw.op("act", ACT(sqq[:, cc, :], p[:], AF.Square), reads=[Rp], wadd=[R_cq])
                    fw.stage("D1")
                    p, Rp = getps()
                    for cc in range(2):
                        fw.op("pe", MM(p[:], onesb[:], sqq[:, cc, :], start=(cc == 0), stop=(cc == 1)),
                              reads=[R_cq, R_C], writes=[Rp] if cc == 0 else [], wadd=[Rp] if cc else [])
                    fw.stage("D1b")
                    rstd_act(rq[:], p[:], Rp, R_rq, 256)
                    fw.stage("D2")
                    for hh in range(4):
                        pa, Rpa = getps()
                        pr, Rpr = getps()
                        for cc in range(2):
                            fw.op("pe", MM(pa[0:96, :], wuqb[:, cc, hh * 192:hh * 192 + 96], cqn[:, cc, :],
                                           start=(cc == 0), stop=(cc == 1)),
                                  reads=[R_W, R_cq], writes=[Rpa] if cc == 0 else [], wadd=[Rpa] if cc else [])
                        for cc in range(2):
                            fw.op("pe", MM(pr[0:96, :], wuqb[:, cc, hh * 192 + 96:hh * 192 + 192], cqn[:, cc, :],
                                           start=(cc == 0), stop=(cc == 1)),
                                  reads=[R_W, R_cq], writes=[Rpr] if cc == 0 else [], wadd=[Rpr] if cc else [])
                        fw.stage("D3")
                        fw.op("dve", STT(qa[0:96, :], pa[0:96, :], SCALE_B, rq[0:96, :], ALU.mult, ALU.mult),
                              reads=[Rpa, R_rq], writes=[R_qa])
                        fw.op("dve", STT(qr[64:96, :], pr[64:96, :], SCALE_B, rq[64:96, :], ALU.mult, ALU.mult),
                              reads=[Rpr, R_rq], wadd=[R_qa])
                        fw.op("dve", TT(qa[64:96, :], qa[64:96, :], cosb[64:96, :], ALU.mult), reads=[R_qa, R_cs], writes=[R_qa])
                        fw.op("dve", TT(qr[64:96, :], qr[64:96, :], sinb[64:96, :], ALU.mult), reads=[R_qa, R_cs], writes=[R_qa])
                        fw.op("dve", TT(qa[64:96, :], qa[64:96, :], qr[64:96, :], ALU.add), reads=[R_qa], writes=[R_qa])
                        s_, Rs_ = getstg()
                        fw.op("act", ACT(s_[0:96, :], qa[0:96, :], AF.Copy), reads=[R_qa], writes=[Rs_])
                        fw.dma("sp", QT[12 + hh, 0:96, t0:t0 + 512], s_[0:96, :], reads=[Rs_], wadd=[R_QT[12 + hh]])
                    fw.stage("E")
                    p, Rp = fchunk(hTg, RhTg, 14 * 128, 128)
                    fw.op("dve", TS(ckvn[:], p[:], kvng[:, 0:1], None, ALU.mult), reads=[Rp, R_C], writes=[R_ckv])
                    fw.op("act", ACT(sqk[:], p[:], AF.Square), reads=[Rp], wadd=[R_ckv])
                    p, Rp = getps()
                    fw.op("pe", MM(p[:], onesb[:], sqk[:]), reads=[R_ckv, R_C], writes=[Rp])
                    rstd_act(rk[:], p[:], Rp, R_rk, 128)
                    p, Rp = getps()
                    for i in range(4):
                        fw.op("pe", MM(p[:, 2 * i:2 * i + 2], sqk[:, i * 128:(i + 1) * 128], onesb[:, 0:2]),
                              reads=[R_ckv, R_C], writes=[Rp] if i == 0 else [], wadd=[Rp] if i else [])
                    rstd_act(rkt[:], p[:, 0:8], Rp, R_rkt, 128)
                    for pair in range(2):
                        p, Rp = getps()
                        fw.op("pe", MM(p[:], wkb[:, pair * 128:(pair + 1) * 128], ckvn[:]), reads=[R_W, R_ckv], writes=[Rp])
                        s_, Rs_ = getstg()
                        fw.op("dve", TT(s_[:], p[:], rk[:], ALU.mult), reads=[Rp, R_rk], writes=[Rs_])
                        for hh in range(2):
                            hd = 12 + 2 * pair + hh
                            fw.dma("sp", KT[hd, 0:64, t0:t0 + 512], s_[hh * 64:(hh + 1) * 64, :], reads=[Rs_],
                                   wadd=[R_KT[hd]])
                    yield
                    fw.stage("F")
                    pk, Rpk = getps()
                    pk2, Rpk2 = getps()
                    for k in range(8):
                        fw.op("pe", MM(pk[0:96, :], winb[:, k, 1920:2016], hTg[:, k, :], start=(k == 0), stop=(k == 7)),
                              reads=[R_W, RhTg], writes=[Rpk] if k == 0 else [], wadd=[Rpk] if k else [])
                    for k in range(8):
                        fw.op("pe", MM(pk2[0:96, :], winb[:, k, 2016:2112], hTg[:, k, :], start=(k == 0), stop=(k == 7)),
                              reads=[R_W, RhTg], writes=[Rpk2] if k == 0 else [], wadd=[Rpk2] if k else [])
                    fw.op("dve", TT(qa[64:96, :], pk[64:96, :], cosb[64:96, :], ALU.mult), reads=[Rpk, R_cs], writes=[R_qa])
                    fw.op("dve", TT(qr[64:96, :], pk2[64:96, :], sinb[64:96, :], ALU.mult), reads=[Rpk2, R_cs], wadd=[R_qa])
                    s_, Rs_ = getstg()
                    fw.op("dve", TT(s_[64:96, :], qa[64:96, :], qr[64:96, :], ALU.add), reads=[R_qa], writes=[Rs_])
                    for hh in range(4):
                        fw.dma("sp", KT[12 + hh, 64:96, t0:t0 + 512], s_[64:96, :], reads=[Rs_], wadd=[R_KT[12 + hh]])
                    pf, Rpf = getps()
                    for k in range(8):
                        fw.op("pe", MM(pf[0:6, :], winb[:, k, 2112:2118], hTg[:, k, :], start=(k == 0), stop=(k == 7)),
                              reads=[R_W, RhTg], writes=[Rpf] if k == 0 else [], wadd=[Rpf] if k else [])
                    fw.op("act", ACT(fe[:], pf[0:6, :], AF.Exp, scale=-1.0, bias=negbf[:, l:l + 1]), reads=[Rpf, R_C], writes=[R_f])
                    fw.op("act", ACT(fe[:], fe[:], AF.Ln, bias=onesf[0:6, 0:1]), reads=[R_f, R_C], writes=[R_f])
                    fw.op("dve", lambda e: e.tensor_tensor_scan(out=fsc[:], data0=onesf[0:6, :], data1=fe[:], initial=fcar[:, 0:1],
                                                                op0=ALU.mult, op1=ALU.add), reads=[R_f, R_C], writes=[R_f])
                    fw.op("dve", CP(fcar[:], fsc[:, 511:512]), reads=[R_f], writes=[R_f])
                    fw.op("dve", CP(fsp[:, 0, :], fsc[:]), reads=[R_f], writes=[R_fs])
                    fw.op("dve", TT(fr[:], fsc[:], fsp[:, 0, :], ALU.subtract), reads=[R_f, R_fs], writes=[R_f])
                    fw.op("dve", CP(fsp[:, 1, :], fr[:]), reads=[R_f], writes=[R_fs])
                    fw.op("dve", TT(fr[:], fr[:], fsp[:, 1, :], ALU.subtract), reads=[R_f, R_fs], writes=[R_f])
                    fw.op("dve", CP(fsp[:, 2, :], fr[:]), reads=[R_f], writes=[R_fs])
                    fw.op("dve", TS(fsn[:].rearrange("p a n -> p (a n)"), fsp[:].rearrange("p a n -> p (a n)"), -1.0, None, ALU.mult),
                          reads=[R_fs], writes=[R_fs])
                    for h in range(6):
                        fw.dma("sp", KT[6 + h:7 + h, 67:70, t0:t0 + 512], fsp[h:h + 1, :, :], reads=[R_fs], wadd=[R_KT[6 + h]])
                        fw.dma("sp", QT[6 + h:7 + h, 64:67, t0:t0 + 512], fsn[h:h + 1, :, :], reads=[R_fs], wadd=[R_QT[6 + h]])
                    fw.stage("G")
                    for i in range(4):
                        ti = g * 4 + i
                        vb, Rvb = vst[ti % 2], R_vst[ti % 2]
                        for (c0, h0) in ((VOFF, 0), (VOFF + 384, 6)):
                            p, Rp = getps()
                            for k in range(8):
                                fw.op("pe", MM(p[:, 0:384], hTg[:, k, i * 128:(i + 1) * 128], winb[:, k, c0:c0 + 384],
                                               start=(k == 0), stop=(k == 7)),
                                      reads=[R_W, RhTg], writes=[Rp] if k == 0 else [], wadd=[Rp] if k else [])
                            kw = dict(writes=[Rvb]) if h0 == 0 else dict(wadd=[Rvb])
                            fw.op("act", ACT(vb[:, h0:h0 + 6, 0:64], p[:, 0:384].rearrange("p (h d) -> p h d", h=6), AF.Copy),
                                  reads=[Rp], **kw)
                        p, Rp = getps()
                        fw.op("pe", MM(p[:, 0:256], ckvn[:, i * 128:(i + 1) * 128], wvb[:]), reads=[R_W, R_ckv], writes=[Rp])
                        fw.op("dve", TS(vb[:, 12:16, 0:64], p[:, 0:256].rearrange("p (h d) -> p h d", h=4),
                                        rkt[:, 2 * i:2 * i + 1], None, ALU.mult), reads=[Rp, R_rkt], wadd=[Rvb])
                        fw.dma("sp", VA[ti * 128:(ti + 1) * 128, :], vb[:].rearrange("p h d -> p (h d)"), reads=[Rvb],
                               wadd=[R_VA])

                load_x(0)
                for i in range(4):
                    front_norm(i)
                    front_tr(i)
                for g in range(NG):
                    gen = proj(g)
                    for i in range(4):
                        ti = (g + 1) * 4 + i
                        if g + 1 < NG:
                            front_norm(ti)
                        next(gen, None)
                        if g + 1 < NG:
                            front_tr(ti)
                    for _ in gen:
                        pass
                fw.flush()

            with ExitStack() as ph:
                Gb = sbuf(ph, "Gb", [128, 6, GW], BF16)
                R_G = Res()
                KTs = [sbuf(ph, "KTs%d" % i, [128, S], BF16) for i in range(2)]
                QTs = [sbuf(ph, "QTs%d" % i, [128, S], BF16) for i in range(2)]
                R_KQ = [Res() for _ in range(2)]
                Vs = [sbuf(ph, "Vs%d" % i, [128, NT * 396 + 128], BF16) for i in range(2)]
                R_Vs = [Res() for _ in range(2)]
                pT = [sbuf(ph, "pT%d" % i, [128, 512], BF16) for i in range(4)]
                R_pT = [Res() for _ in range(4)]
                rden = [sbuf(ph, "rden%d" % i, [128, 512], F32) for i in range(2)]
                bc = [sbuf(ph, "bc%d" % i, [64, 512], F32) for i in range(2)]
                ost = [sbuf(ph, "ost%d" % i, [64, 512], BF16) for i in range(2)]
                R_fin = [Res() for _ in range(2)]
                for h in range(6):
                    fw.dma("pool", Gb[:, h, :], Gd[:, h, :], wadd=[R_G])
                for i in range(2):
                    fw.op("pool", MS(rden[i][:], 0.0), writes=[R_fin[i]])
                    fw.op("pool", MS(Vs[i][:], 0.0), writes=[R_Vs[i]])
                    fw.op("pool", MS(KTs[i][:], 0.0), writes=[R_KQ[i]])
                heads = []
                for h in range(6):
                    heads.append((h, 80, 0, h, h * 64, "A"))
                for h in range(6):
                    heads.append((6 + h, 70, 1, h, 640 + h * 64, "C"))
                for h in range(4):
                    heads.append((12 + h, 96, 2, h, 384 + h * 64, "B"))
                vg_range = {0: (0, 6), 1: (6, 6), 2: (12, 4)}

                def load_head(n):
                    hd, Kc, vg, vi, row0, ty = heads[n]
                    b = n % 2
                    fw.op("pool", MS(QTs[b][64:128, :], 0.0), writes=[R_KQ[b]])
                    fw.dma("sp", QTs[b][0:Kc, :], QT[hd, 0:Kc, :], reads=[R_QT[hd]], writes=[R_KQ[b]])
                    fw.dma("sp", KTs[b][0:Kc, :], KT[hd, 0:Kc, :], reads=[R_KT[hd]], wadd=[R_KQ[b]])

                def load_v(vg):
                    h0, nh = vg_range[vg]
                    b = vg % 2
                    fw.dma("sp", Vs[b][:, 0:NT * 396].rearrange("p (t c) -> p t c", c=396)[:, :, 0:nh * 66],
                           VA[:, h0 * 66:(h0 + nh) * 66].rearrange("(t p) c -> p t c", p=128),
                           reads=[R_VA], writes=[R_Vs[b]])

                tasks = []
                for n, hdinfo in enumerate(heads):
                    for g in range(NG):
                        for kt in range(4 * (g + 1)):
                            tasks.append((n, g, kt))
                load_head(0)
                load_v(0)
                srr = [0]
                state = {}

                def emit_qk(tk):
                    n, g, kt = tk
                    hd, Kc, vg, vi, row0, ty = heads[n]
                    b = n % 2
                    si = srr[0]
                    srr[0] = (si + 1) % 4
                    p, Rp = PS[si], R_PS[si]
                    j = kt - 4 * g
                    c0 = 128 * j if j >= 0 else 0
                    extra = (ty == "A") or (j >= 0)
                    fw.op("pe", MM(p[:, c0:512], KTs[b][:, kt * 128:(kt + 1) * 128], QTs[b][:, g * 512 + c0:(g + 1) * 512],
                                   start=True, stop=not extra), reads=[R_KQ[b]], writes=[Rp])
                    if ty == "A":
                        off = min(512 * g - 128 * kt, 1024) + 384
                        fw.op("pe", MM(p[:, c0:512], identb[:], Gb[:, vi, off + c0:off + 512], start=False, stop=True),
                              reads=[R_G, R_C], wadd=[Rp])
                    elif j >= 0:
                        fw.op("pe", MM(p[:, c0:c0 + 128], identb[:], trib[:], start=False, stop=True), reads=[R_C], wadd=[Rp])
                    if ty != "A" and WARM:
                        fw.op("pe", MM(PS[6][:, :], identb[:], Gb[:, 0, 0:512]), reads=[R_G, R_C], writes=[R_PS[6]])
                    fw.op("act", ACT(pT[si][:, c0:512], p[:, c0:512], AF.Exp), reads=[Rp], writes=[R_pT[si]])
                    state[tk] = (si, c0)

                orr = [0]
                ostate = {}

                def emit_pv(tk):
                    n, g, kt = tk
                    hd, Kc, vg, vi, row0, ty = heads[n]
                    si, c0 = state.pop(tk)
                    if kt == 0:
                        oi = orr[0]
                        orr[0] = (oi + 1) % 2
                        ostate[(n, g)] = oi
                        for ff, age in [x for x in pend if x[0][2] == oi]:
                            emit_fin(ff)
                            emit_fin2(ff)
                        pend[:] = [x for x in pend if x[0][2] != oi]
                        for ff, age in [x for x in pend2 if x[0][2] == oi]:
                            emit_fin2(ff)
                        pend2[:] = [x for x in pend2 if x[0][2] != oi]
                    oi = ostate[(n, g)]
                    po, Rpo = PS[4 + oi], R_PS[4 + oi]
                    last = (kt == 4 * (g + 1) - 1)
                    vb0 = kt * 396 + vi * 66
                    fw.op("pe", MM(po[:, c0:512], Vs[vg % 2][:, vb0:vb0 + 128], pT[si][:, c0:512],
                                   start=(kt == 0), stop=last),
                          reads=[R_Vs[vg % 2], R_pT[si]], writes=[Rpo] if kt == 0 else [], wadd=[Rpo] if kt else [])
                    if last:
                        fw.op("dve", lambda e, oi=oi, po=po: e.reciprocal(out=rden[oi][64:65, :], in_=po[64:65, :]),
                              reads=[Rpo], writes=[R_fin[oi]])
                        return (n, g, oi)
                    return None

                def emit_fin(f):
                    n, g, oi = f
                    pb, Rpb = PS[6], R_PS[6]
                    fw.op("pe", MM(pb[0:64, :], selrow[:], rden[oi][:]), reads=[R_fin[oi], R_C], writes=[Rpb])

                def emit_fin2(f):
                    n, g, oi = f
                    hd, Kc, vg, vi, row0, ty = heads[n]
                    po, Rpo = PS[4 + oi], R_PS[4 + oi]
                    pb, Rpb = PS[6], R_PS[6]
                    fw.op("dve", CP(bc[oi][:], pb[0:64, :]), reads=[Rpb], wadd=[R_fin[oi]])
                    fw.op("dve", TT(ost[oi][:], po[0:64, :], bc[oi][:], ALU.mult), reads=[Rpo, R_fin[oi]], wadd=[R_fin[oi]])
                    fw.dma("sp", MT[row0:row0 + 64, g * 512:(g + 1) * 512], ost[oi][:], reads=[R_fin[oi]], wadd=[R_MT])

                pend = []
                pend2 = []
                LOOK = 2
                for i0 in range(min(LOOK, len(tasks))):
                    emit_qk(tasks[i0])
                for idx, tk in enumerate(tasks):
                    n, g, kt = tk
                    if g == 0 and kt == 0:
                        if n + 1 < len(heads):
                            load_head(n + 1)
                            if heads[n + 1][2] != heads[n][2]:
                                load_v(heads[n + 1][2])
                    if idx + LOOK < len(tasks):
                        emit_qk(tasks[idx + LOOK])
                    f = emit_pv(tk)
                    pend[:] = [(ff, age + 1) for (ff, age) in pend]
                    pend2[:] = [(ff, age + 1) for (ff, age) in pend2]
                    while pend2 and pend2[0][1] >= 2:
                        emit_fin2(pend2.pop(0)[0])
                    while pend and pend[0][1] >= 5:
                        ff = pend.pop(0)[0]
                        emit_fin(ff)
                        pend2.append((ff, 0))
                    if f is not None:
                        pend.append((f, 0))
                for ff, age in pend2:
                    emit_fin2(ff)
                for ff, age in pend:
                    emit_fin(ff)
                    emit_fin2(ff)
                fw.flush()

            with ExitStack() as ph:
                wob = sbuf(ph, "wob", [128, 8, D], BF16)
                R_W = Res()
                gpost = sbuf(ph, "gpost", [128, D], F32)
                mT = [sbuf(ph, "mT%d" % i, [128, 8, 512], BF16) for i in range(2)]
                R_mT = [Res() for _ in range(2)]
                xin = [sbuf(ph, "xin%d" % i, [128, D], F32) for i in range(2)]
                R_xin = [Res() for _ in range(2)]
                junk = sbuf(ph, "junk", [128, 512], BF16)
                R_junk = Res()
                stat = [sbuf(ph, "stat%d" % i, [128, 8], F32) for i in range(2)]
                R_stat = [Res() for _ in range(2)]
                yo = [sbuf(ph, "yo%d" % i, [128, D], F32) for i in range(2)]
                R_yo = [Res() for _ in range(2)]
                fw.dma("pool", wob[:], w_o[l].rearrange("(k p) n -> p k n", p=128), writes=[R_W])
                fw.dma("sp", gpost[:], g_mixpost[l], wadd=[R_W])

                def load_m(g):
                    fw.dma("sp", mT[g % 2][:], MT[:, g * 512:(g + 1) * 512].rearrange("(k p) n -> p k n", p=128),
                           reads=[R_MT], writes=[R_mT[g % 2]])
                load_m(0)
                for g in range(NG):
                    if g + 1 < NG:
                        load_m(g + 1)
                    for i in range(4):
                        ti = g * 4 + i
                        b = ti % 2
                        fw.dma("sp", xin[b][:], xsrc[ti * 128:(ti + 1) * 128, :], reads=[Rxsrc], writes=[R_xin[b]])
                        pp = []
                        for half in range(2):
                            p, Rp = getps()
                            pp.append((p, Rp))
                            for k in range(8):
                                fw.op("pe", MM(p[:], mT[g % 2][:, k, i * 128:(i + 1) * 128], wob[:, k, half * 512:(half + 1) * 512],
                                               start=(k == 0), stop=(k == 7)),
                                      reads=[R_W, R_mT[g % 2]], writes=[Rp] if k == 0 else [], wadd=[Rp] if k else [])
                        for half in range(2):
                            p, Rp = pp[half]
                            fw.op("act", ACT(junk[:], p[:], AF.Square, accum_out=stat[b][:, half:half + 1]),
                                  reads=[Rp], writes=[R_junk], wadd=[R_stat[b]])
                        fw.op("dve", TT(stat[b][:, 2:3], stat[b][:, 0:1], stat[b][:, 1:2], ALU.add), reads=[R_stat[b], R_junk],
                              writes=[R_stat[b]])
                        rms_rstd(stat[b][:, 2:3], stat[b][:, 3:4], R_stat[b], D)
                        for half in range(2):
                            p, Rp = pp[half]
                            sl = slice(half * 512, (half + 1) * 512)
                            fw.op("dve", STT(yo[b][:, sl], p[:], stat[b][:, 3:4], gpost[:, sl], ALU.mult, ALU.mult),
                                  reads=[Rp, R_stat[b], R_W], **(dict(writes=[R_yo[b]]) if half == 0 else dict(wadd=[R_yo[b]])))
                        fw.op("pool", TT(yo[b][:], yo[b][:], xin[b][:], ALU.add), reads=[R_yo[b], R_xin[b]], writes=[R_yo[b]])
                        fw.dma("sp", X1[ti * 128:(ti + 1) * 128, :], yo[b][:], reads=[R_yo[b]], wadd=[R_X1])
                fw.flush()

            with ExitStack() as ph:
                wupb = sbuf(ph, "wupb", [128, 8, 2 * DFF], BF16)
                wdnb = sbuf(ph, "wdnb", [128, 22, D], BF16)
                R_W = Res()
                gpre = sbuf(ph, "gpre", [128, 8], F32)
                gpost = sbuf(ph, "gpost", [128, D], F32)
                cvp = sbuf(ph, "cvp", [128, NCH_UP, 4], F32)
                fw.dma("sp", gpre[:], g_ffnpre[l], writes=[R_C])
                fw.dma("sp", gpost[:], g_ffnpost[l], wadd=[R_C])
                fw.dma("sp", cvp[:], convp[l], wadd=[R_C])
                with ExitStack() as ph2:
                    wst = [sbuf(ph2, "wst%d" % i, [128, 1408], F32) for i in range(2)]
                    R_wst = [Res() for _ in range(2)]
                    ctr = [0]
                    for k in range(8):
                        load_scaled_weight(wst, R_wst, ctr, wupb[:, k, :], w_up[l, k * 128:(k + 1) * 128, :], gpre[:, k:k + 1], R_W)
                    fw.dma("pool", wdnb[:, 0:11, :], w_down[l, 0:1408, :].rearrange("(c p) n -> p c n", p=128), wadd=[R_W])
                    fw.dma("pool", wdnb[:, 11:22, :], w_down[l, 1408:2816, :].rearrange("(c p) n -> p c n", p=128), wadd=[R_W])
                    fw.flush()
                xin = [sbuf(ph, "xin%d" % i, [128, D], F32) for i in range(4)]
                R_xin = [Res() for _ in range(4)]
                junk = sbuf(ph, "junk", [128, D], BF16)
                R_junk = Res()
                stat = [sbuf(ph, "stat%d" % i, [128, 8], F32) for i in range(2)]
                R_stat = [Res() for _ in range(2)]
                hb = [sbuf(ph, "hb%d" % i, [128, D], BF16) for i in range(2)]
                R_hb = [Res() for _ in range(2)]
                hT = [sbuf(ph, "hT%d" % i, [128, 8, 256], BF16) for i in range(2)]
                R_hT = [Res() for _ in range(2)]
                aT = sbuf(ph, "aT", [128, 22, 256], BF16)
                R_aT = Res()
                NUE = 4
                uext = [sbuf(ph, "uext%d" % i, [128, 258], F32) for i in range(NUE)]
                R_ue = [Res() for _ in range(NUE)]
                acc = [sbuf(ph, "acc%d" % i, [128, 256], F32) for i in range(NUE)]
                R_acc = [Res() for _ in range(NUE)]
                gl = [sbuf(ph, "gl%d" % i, [128, 256], F32) for i in range(2)]
                R_gl = [Res() for _ in range(2)]
                halo = sbuf(ph, "halo", [128, NCH_UP, 2], F32)
                R_halo = [Res() for _ in range(NCH_UP)]
                yo = [sbuf(ph, "yo%d" % i, [128, D], F32) for i in range(2)]
                R_yo = [Res() for _ in range(2)]
                fw.op("pool", MS(halo[:], 0.0), writes=R_halo)
                NG2 = S // 256

                def load_x4(ti):
                    b = ti % 4
                    fw.dma("sp", xin[b][:], X1[ti * 128:(ti + 1) * 128, :], reads=[R_X1], writes=[R_xin[b]])
                def p4_norm(ti):
                    norm_part(xin[ti % 4], R_xin[ti % 4], junk, R_junk, stat[ti % 2], R_stat[ti % 2], hb[ti % 2], R_hb[ti % 2])

                def p4_tr(ti):
                    gg, i = ti // 2, ti % 2
                    tr_part(hb[ti % 2], R_hb[ti % 2], hT[gg % 2], R_hT[gg % 2], i * 128, i == 0)

                load_x4(0)
                load_x4(1)
                for i in range(2):
                    p4_norm(i)
                    p4_tr(i)
                for g in range(NG2):
                    hTg, RhTg = hT[g % 2], R_hT[g % 2]
                    nxt = g + 1 < NG2
                    if nxt:
                        load_x4(2 * g + 2)
                        load_x4(2 * g + 3)
                    for c in range(22):
                        if nxt and c == 0:
                            p4_norm(2 * g + 2)
                        if nxt and c == 4:
                            p4_tr(2 * g + 2)
                        if nxt and c == 8:
                            p4_norm(2 * g + 3)
                        if nxt and c == 12:
                            p4_tr(2 * g + 3)
                        res = []
                        for which in range(2):
                            ch = c + 22 * which
                            p, Rp = getps()
                            for k in range(8):
                                fw.op("pe", MM(p[:, 0:256], wupb[:, k, ch * 128:(ch + 1) * 128], hTg[:, k, :], start=(k == 0), stop=(k == 7)),
                                      reads=[R_W, RhTg], writes=[Rp] if k == 0 else [], wadd=[Rp] if k else [])
                            bi = (2 * c + which) % NUE
                            ue, Rue = uext[bi], R_ue[bi]
                            ac, Rac = acc[bi], R_acc[bi]
                            fw.op("act", ACT(ue[:, 2:258], p[:, 0:256], AF.Copy), reads=[Rp], writes=[Rue])
                            fw.op("pool", CP(ue[:, 0:2], halo[:, ch, :]), reads=[R_halo[ch]], wadd=[Rue])
                            fw.op("act", ACT(ac[:], p[:, 0:256], AF.Identity, scale=cvp[:, ch, 2:3], bias=cvp[:, ch, 3:4]),
                                  reads=[Rp, R_C], writes=[Rac])
                            fw.op("pool", CP(halo[:, ch, :], ue[:, 256:258]), reads=[Rue], writes=[R_halo[ch]])
                            fw.op("dve", STT(ac[:], ue[:, 1:257], cvp[:, ch, 1:2], ac[:], ALU.mult, ALU.add), reads=[Rue, Rac, R_C], writes=[Rac])
                            fw.op("dve", STT(ac[:], ue[:, 0:256], cvp[:, ch, 0:1], ac[:], ALU.mult, ALU.add), reads=[Rue, Rac, R_C], writes=[Rac])
                        ag, av = (2 * c) % NUE, (2 * c + 1) % NUE
                        fw.op("act", ACT(gl[c % 2][:], acc[ag][:], AF.Gelu_apprx_tanh), reads=[R_acc[ag]], writes=[R_gl[c % 2]])
                        fw.op("dve", TT(aT[:, c, :], gl[c % 2][:], acc[av][:], ALU.mult), reads=[R_gl[c % 2], R_acc[av]],
                              **(dict(writes=[R_aT]) if c == 0 else dict(wadd=[R_aT])))
                    for i in range(2):
                        ti = g * 2 + i
                        b = ti % 2
                        xb, Rxb = xin[ti % 4], R_xin[ti % 4]
                        pp = []
                        for half in range(2):
                            p, Rp = getps()
                            pp.append((p, Rp))
                            for c in range(22):
                                fw.op("pe", MM(p[:], aT[:, c, i * 128:(i + 1) * 128], wdnb[:, c, half * 512:(half + 1) * 512],
                                               start=(c == 0), stop=(c == 21)),
                                      reads=[R_W, R_aT], writes=[Rp] if c == 0 else [], wadd=[Rp] if c else [])
                        for half in range(2):
                            p, Rp = pp[half]
                            fw.op("act", ACT(junk[:, 0:512], p[:], AF.Square, accum_out=stat[b][:, 4 + half:5 + half]),
                                  reads=[Rp], writes=[R_junk], wadd=[R_stat[b]])
                        fw.op("dve", TT(stat[b][:, 6:7], stat[b][:, 4:5], stat[b][:, 5:6], ALU.add), reads=[R_stat[b], R_junk],
                              writes=[R_stat[b]])
                        rms_rstd(stat[b][:, 6:7], stat[b][:, 7:8], R_stat[b], D)
                        for half in range(2):
                            p, Rp = pp[half]
                            sl = slice(half * 512, (half + 1) * 512)
                            fw.op("dve", STT(yo[b][:, sl], p[:], stat[b][:, 7:8], gpost[:, sl], ALU.mult, ALU.mult),
                                  reads=[Rp, R_stat[b], R_C], **(dict(writes=[R_yo[b]]) if half == 0 else dict(wadd=[R_yo[b]])))
                        fw.op("pool", TT(yo[b][:], yo[b][:], xb[:], ALU.add), reads=[R_yo[b], Rxb], writes=[R_yo[b]])
                        fw.dma("sp", xdst[ti * 128:(ti + 1) * 128, :], yo[b][:], reads=[R_yo[b]], wadd=[Rxdst])
                fw.flush()
    return nc


def _t5_bucket_np(n):
    n = np.maximum(n, 0)
    exact = 16
    large = exact + (np.log(np.maximum(n, 1).astype(np.float32) / exact) / math.log(1024 / exact) * (32 - exact)).astype(np.int32)
    return np.where(n < exact, n, np.minimum(large, 31))


def host_consts(S):
    c = np.arange(128)[:, None]
    m = np.arange(GW)[None, :]
    r = m - 384 - c
    bidx = _t5_bucket_np(r).astype(np.int64)
    causal = r >= 0
    tri = np.where(np.arange(128)[None, :] >= np.arange(128)[:, None], 0.0, -BIG).astype(np.float32)
    half = 16
    inv = (10000.0 ** (-np.arange(half, dtype=np.float32) / half)).astype(np.float32)
    ang = np.arange(S, dtype=np.float32)[None, :] * inv[:, None]
    cos, sin = np.cos(ang).astype(np.float32), np.sin(ang).astype(np.float32)
    cosT = np.zeros((128, S), np.float32)
    sinS = np.zeros((128, S), np.float32)
    cosT[64:80] = cos
    cosT[80:96] = cos
    sinS[64:80] = -sin
    sinS[80:96] = sin
    blk = (np.arange(S)[None, :] // 256 == np.arange(16)[:, None]).astype(np.float32)
    return bidx, causal, tri, cosT, sinS, blk


def host_layout(inp, S, L):
    bidx, causal, tri, cosT, sinS, blk = host_consts(S)
    rb = np.asarray(inp["rel_bias"], np.float32)
    G = np.where(causal[:, None, :], rb[bidx].transpose(0, 2, 1), np.float32(-BIG)).astype(np.float32)
    w_in = np.asarray(inp["w_in"], np.float32)
    o = {"a_q": 0, "a_k": 384, "a_v": 768, "c_q": 1152, "c_kv": 1408, "k_r": 1536, "f_q": 1568, "f_k": 1952, "f_v": 2336, "f_g": 2720}
    cols = np.concatenate([
        np.arange(o["a_q"], o["a_q"] + 384), np.arange(o["a_k"], o["a_k"] + 384),
        np.arange(o["f_q"], o["f_q"] + 384), np.arange(o["f_k"], o["f_k"] + 384),
        np.arange(o["c_q"], o["c_q"] + 256), np.arange(o["c_kv"], o["c_kv"] + 128),
        np.full(64, -1), np.arange(o["k_r"], o["k_r"] + 32),
        np.full(64, -1), np.arange(o["k_r"] + 16, o["k_r"] + 32), np.arange(o["k_r"], o["k_r"] + 16),
        np.arange(o["f_g"], o["f_g"] + 6), np.full(2, -1),
        np.arange(o["a_v"], o["a_v"] + 384), np.arange(o["f_v"], o["f_v"] + 384)])
    assert cols.shape[0] == WIN
    w_in_r = np.zeros((L, D, WIN), np.float32)
    w_in_r[:, :, cols >= 0] = w_in[:L][:, :, cols[cols >= 0]]
    w_uq = np.asarray(inp["w_uq"], np.float32)[:L]
    uq = w_uq.reshape(L, 256, 4, 96)
    w_uq_r = np.ascontiguousarray(np.concatenate([uq, np.zeros((L, 256, 4, 64), np.float32), uq[..., 80:96], uq[..., 64:80]],
                                                 axis=-1).reshape(L, 256, 768))
    ukv = np.asarray(inp["w_ukv"], np.float32)[:L].reshape(L, 128, 4, 128)
    w_ukvk = np.ascontiguousarray(ukv[..., :64].reshape(L, 128, 256))
    w_ukvv = np.ascontiguousarray(ukv[..., 64:].reshape(L, 128, 256))

    def pk(v):
        return np.ascontiguousarray(np.asarray(v, np.float32)[:L].reshape(L, 8, 128).transpose(0, 2, 1))

    def bc(v):
        return np.ascontiguousarray(np.broadcast_to(np.asarray(v, np.float32)[:L, None, :], (L, 128, D)))
    cw = np.asarray(inp["conv_w"], np.float32)[:L]
    cb = np.asarray(inp["conv_b"], np.float32)[:L]
    cv = np.concatenate([cw, cb[:, None, :]], axis=1)
    convp = np.ascontiguousarray(cv.reshape(L, 4, NCH_UP, 128).transpose(0, 3, 2, 1))
    return {
        "w_in": w_in_r,
        "g_mixpre": pk(inp["ln_mix_pre"]), "g_ffnpre": pk(inp["ln_ffn_pre"]),
        "g_mixpost": bc(inp["ln_mix_post"]), "g_ffnpost": bc(inp["ln_ffn_post"]),
        "b_f": np.ascontiguousarray(np.asarray(inp["b_f"], np.float32)[:L].T),
        "qn": np.ascontiguousarray(np.asarray(inp["q_norm"], np.float32)[:L].reshape(L, 2, 128).transpose(0, 2, 1)),
        "kvn": np.ascontiguousarray(np.asarray(inp["kv_norm"], np.float32)[:L].reshape(L, 128, 1)),
        "w_uq": w_uq_r, "w_ukvk": w_ukvk, "w_ukvv": w_ukvv,
        "w_o": np.ascontiguousarray(np.asarray(inp["w_o"], np.float32)[:L]),
        "w_up": np.ascontiguousarray(np.asarray(inp["w_up"], np.float32)[:L]),
        "convp": convp,
        "w_down": np.ascontiguousarray(np.asarray(inp["w_down"], np.float32)[:L]),
        "G": np.ascontiguousarray(G), "tri": tri, "ident": np.eye(128, dtype=np.float32),
        "cosT": cosT, "sinS": sinS, "blk1h": blk,
    }


_NC_CACHE = {}


def kernel(**inputs):
    x = np.asarray(inputs["x"], np.float32)
    B, S, _ = x.shape
    L = np.asarray(inputs["w_in"]).shape[0]
    shared = host_layout(inputs, S, L)
    key = (S, L)
    if key not in _NC_CACHE:
        _NC_CACHE[key] = build(S, L)
    nc = _NC_CACHE[key]
    in_maps = []
    for b in range(B):
        m = dict(shared)
        m["x"] = np.ascontiguousarray(x[b])
        in_maps.append(m)
    res = run_bass_kernel_spmd(nc, in_maps, core_ids=list(range(B)))
    return np.stack([np.asarray(r["out"], np.float32) for r in res.results], axis=0)
```

```python
import math
import os
from contextlib import ExitStack
import numpy as np
import concourse.bass as bass
import concourse.mybir as mybir
from concourse.bass_utils import run_bass_kernel_spmd

F32 = mybir.dt.float32
BF16 = mybir.dt.bfloat16
AF = mybir.ActivationFunctionType
ALU = mybir.AluOpType
AX = mybir.AxisListType

D = 1024
DFF = 2816
NCH_UP = 44
BIG = 30000.0
EPS = 1e-6
WIN = 2888
VOFF = 2120
GW = 1920
SCALE_A = 64 ** -0.5
SCALE_B = 96 ** -0.5
WARM = False


class Res:
    __slots__ = ("name", "w", "r", "pw", "pr", "excl")

    def __init__(self, name="", excl=False):
        self.name = name
        self.excl = excl
        self.w = {}
        self.r = {}
        self.pw = {}
        self.pr = {}


class FW:
    NDMA = 8

    def __init__(self, nc, stack):
        self.nc = nc
        self.eng = ("pe", "dve", "act", "pool", "sp")
        self.sems = {}
        self.cnt = {}
        for e in ("pe", "dve", "act", "pool"):
            self.sems[e] = stack.enter_context(nc.semaphore("s_" + e))
            self.cnt[e] = 0
        self.drr = {}
        for q in ("sp", "pool"):
            self.drr[q] = 0
            for i in range(self.NDMA):
                k = "d_%s%d" % (q, i)
                self.sems[k] = stack.enter_context(nc.semaphore(k))
                self.cnt[k] = 0
        self.seen = {e: {} for e in self.eng}
        self.streams = {e: [] for e in self.eng}
        self.nops = 0
        self.nflush = 0
        self.stop = None
        self.dead = False
        self.killed = False
        self.stop_stage = None

    def stage(self, name):
        if name == self.stop_stage:
            self.dead = True

    def _wait(self, e, deps):
        seen = self.seen[e]
        for k, v in deps.items():
            if e == "pe" and k == "pe":
                continue
            if seen.get(k, 0) < v:
                self.streams[e].append(("w", self.sems[k], v))
                seen[k] = v

    def _deps(self, reads, writes, wadd):
        deps = {}

        def add(k, v):
            if deps.get(k, 0) < v:
                deps[k] = v
        for t in reads:
            for k, v in t.w.items():
                add(k, v)
            if t.excl:
                for k, v in t.r.items():
                    add(k, v)
        for t in writes:
            for k, v in t.w.items():
                add(k, v)
            for k, v in t.r.items():
                add(k, v)
        for t in wadd:
            for k, v in t.r.items():
                add(k, v)
            for k, v in t.pw.items():
                add(k, v)
            for k, v in t.pr.items():
                add(k, v)
        return deps

    def _mark(self, k, v, reads, writes, wadd):
        for t in reads:
            if t.r.get(k, 0) < v:
                t.r[k] = v
        for t in writes:
            t.pw = t.w
            t.pr = t.r
            t.w = {k: v}
            t.r = {}
        for t in wadd:
            t.w[k] = v

    def op(self, e, fn, reads=(), writes=(), wadd=()):
        if self.dead:
            return
        self._wait(e, self._deps(reads, writes, wadd))
        self.streams[e].append(("i", fn, self.sems[e], 1))
        self.cnt[e] += 1
        self._mark(e, self.cnt[e], reads, writes, wadd)
        self.nops += 1

    def dma(self, q, out, in_, reads=(), writes=(), wadd=()):
        if self.dead:
            return
        i = self.drr[q]
        self.drr[q] = (i + 1) % self.NDMA
        k = "d_%s%d" % (q, i)
        deps = self._deps(reads, writes, wadd)
        if self.cnt[k] > 0 and deps.get(k, 0) < self.cnt[k]:
            deps[k] = self.cnt[k]
        self._wait(q, deps)
        self.streams[q].append(("i", (lambda e, out=out, in_=in_: e.dma_start(out=out, in_=in_)), self.sems[k], 16))
        self.cnt[k] += 16
        self._mark(k, self.cnt[k], reads, writes, wadd)
        self.nops += 1

    def flush(self):
        if self.killed:
            return
        self.nflush += 1
        if self.dead or (self.stop is not None and self.nflush >= self.stop):
            self.dead = True
            self.killed = True
        deps = {k: v for k, v in self.cnt.items() if v > 0}
        for e in self.eng:
            self._wait(e, deps)
        streams = self.streams

        def rp(lst):
            def f(eng):
                for it in lst:
                    if it[0] == "w":
                        eng.wait_ge(it[1], it[2])
                    else:
                        it[1](eng).then_inc(it[2], it[3])
            return f
        with self.nc.Block() as block:
            block.sync(rp(streams["sp"]))
            block.tensor(rp(streams["pe"]))
            block.vector(rp(streams["dve"]))
            block.scalar(rp(streams["act"]))
            block.gpsimd(rp(streams["pool"]))
        self.streams = {e: [] for e in self.eng}


def MM(out, lhsT, rhs, start=True, stop=True):
    return lambda e: e.matmul(out, lhsT=lhsT, rhs=rhs, start=start, stop=stop)


def TR(out, in_, ident):
    return lambda e: e.transpose(out=out, in_=in_, identity=ident)


def ACT(out, in_, func, **kw):
    return lambda e: e.activation(out=out, in_=in_, func=func, **kw)


def TS(out, in0, s1, s2, op0, op1=None):
    if op1 is None:
        return lambda e: e.tensor_scalar(out=out, in0=in0, scalar1=s1, scalar2=None, op0=op0)
    return lambda e: e.tensor_scalar(out=out, in0=in0, scalar1=s1, scalar2=s2, op0=op0, op1=op1)


def STT(out, in0, scalar, in1, op0, op1):
    return lambda e: e.scalar_tensor_tensor(out=out, in0=in0, scalar=scalar, in1=in1, op0=op0, op1=op1)


def TT(out, in0, in1, op):
    return lambda e: e.tensor_tensor(out=out, in0=in0, in1=in1, op=op)


def CP(out, in_):
    return lambda e: e.tensor_copy(out=out, in_=in_)


def MS(ap, val):
    return lambda e: e.memset(ap, val)


def build(S, L, dbg=False, stop=None, stop_stage=None):
    NT = S // 128
    NG = S // 512
    NBLK = S // 256
    nc = bass.Bass("TRN2", target_bir_lowering=False)

    def dram(name, shape, dtype, kind="Internal"):
        return nc.dram_tensor(name, shape, dtype, kind=kind).ap()
    EI = "ExternalInput"
    SCR = "ExternalOutput" if dbg else "Internal"
    x_in = dram("x", [S, D], F32, EI)
    out = dram("out", [S, D], F32, "ExternalOutput")
    w_in = dram("w_in", [L, D, WIN], F32, EI)
    g_mixpre = dram("g_mixpre", [L, 128, 8], F32, EI)
    g_ffnpre = dram("g_ffnpre", [L, 128, 8], F32, EI)
    g_mixpost = dram("g_mixpost", [L, 128, D], F32, EI)
    g_ffnpost = dram("g_ffnpost", [L, 128, D], F32, EI)
    b_f = dram("b_f", [6, L], F32, EI)
    qn = dram("qn", [L, 128, 2], F32, EI)
    kvn = dram("kvn", [L, 128, 1], F32, EI)
    w_uq = dram("w_uq", [L, 256, 768], F32, EI)
    w_ukvk = dram("w_ukvk", [L, 128, 256], F32, EI)
    w_ukvv = dram("w_ukvv", [L, 128, 256], F32, EI)
    w_o = dram("w_o", [L, D, D], F32, EI)
    w_up = dram("w_up", [L, D, 2 * DFF], F32, EI)
    convp = dram("convp", [L, 128, NCH_UP, 4], F32, EI)
    w_down = dram("w_down", [L, DFF, D], F32, EI)
    Gd = dram("G", [128, 6, GW], F32, EI)
    trid = dram("tri", [128, 128], F32, EI)
    identd = dram("ident", [128, 128], F32, EI)
    cosd = dram("cosT", [128, S], F32, EI)
    sind = dram("sinS", [128, S], F32, EI)
    blkd = dram("blk1h", [16, S], F32, EI)

    QT = dram("QT", [16, 128, S], BF16, SCR)
    KT = dram("KT", [16, 128, S], BF16, SCR)
    VA = dram("VA", [S, 16 * 66], BF16, SCR)
    MT = dram("MT", [D, S], BF16, SCR)
    X1 = dram("X1", [S, D], F32, SCR)
    XM = dram("XM", [S, D], F32, SCR)

    R_QT = [Res("QT%d" % i) for i in range(16)]
    R_KT = [Res("KT%d" % i) for i in range(16)]
    R_VA = Res("VA")
    R_MT = Res("MT")
    R_X1 = Res("X1")
    R_XM = Res("XM")
    R_OUT = Res("out")
    R_IN = Res("in")

    with ExitStack() as top:
        fw = FW(nc, top)
        fw.stop = stop
        fw.stop_stage = stop_stage

        uniq = [0]

        def sbuf(st, name, shape, dtype):
            uniq[0] += 1
            return st.enter_context(nc.sbuf_tensor("%s_%d" % (name, uniq[0]), shape, dtype))

        PS = [top.enter_context(nc.psum_tensor("ps%d" % i, [128, 512], F32)) for i in range(7)]
        R_PS = [Res("ps%d" % i, excl=True) for i in range(7)]
        PSB = top.enter_context(nc.psum_tensor("psb", [128, 1024], BF16))
        R_PSB = Res("psb", excl=True)
        psrr = [0]

        def getps(lo=0, hi=7):
            i = psrr[0]
            psrr[0] = (i + 1 - lo) % (hi - lo) + lo
            return PS[i], R_PS[i]

        identb = sbuf(top, "identb", [128, 128], BF16)
        trib = sbuf(top, "trib", [128, 128], BF16)
        onesb = sbuf(top, "onesb", [128, 128], BF16)
        onesf = sbuf(top, "onesf", [128, 512], F32)
        mhalf = sbuf(top, "mhalf", [128, 512], F32)
        selrow = sbuf(top, "selrow", [128, 64], F32)
        epsb = sbuf(top, "epsb", [128, 1], F32)
        negbf = sbuf(top, "negbf", [6, L], F32)
        R_C = Res("consts")
        fw.dma("pool", identb[:], identd[:], writes=[R_C])
        fw.dma("pool", trib[:], trid[:], wadd=[R_C])
        fw.op("pool", MS(onesb[:], 1.0), wadd=[R_C])
        fw.op("pool", MS(onesf[:], 1.0), wadd=[R_C])
        fw.op("pool", MS(mhalf[:], -0.5), wadd=[R_C])
        fw.op("dve", MS(selrow[:], 0.0), wadd=[R_C])
        fw.op("dve", MS(epsb[:], EPS), wadd=[R_C])
        fw.op("dve", MS(selrow[64:65, :], 1.0), reads=[R_C], wadd=[R_C])
        fw.dma("sp", negbf[:], b_f[:], wadd=[R_C])
        fw.op("dve", TS(negbf[:], negbf[:], -1.0, None, ALU.mult), reads=[R_C], wadd=[R_C])
        with ExitStack() as ph:
            c1h = sbuf(ph, "c1h", [16, S], BF16)
            c1 = sbuf(ph, "c1", [3, S], BF16)
            R1 = Res()
            fw.dma("pool", c1h[:].rearrange("p (a n) -> p a n", n=512), blkd.rearrange("p (a n) -> p a n", n=512), writes=[R1])
            fw.op("dve", MS(c1[:], 1.0), wadd=[R1])
            for h in range(6):
                fw.dma("sp", KT[h, 64:80, :], c1h[:], reads=[R1], wadd=[R_KT[h]])
                fw.dma("sp", KT[6 + h, 64:67, :], c1[:], reads=[R1], wadd=[R_KT[6 + h]])
                fw.dma("sp", QT[6 + h, 67:70, :], c1[:], reads=[R1], wadd=[R_QT[6 + h]])
            fw.flush()

        def load_scaled_weight(st_tiles, R_st, ctr, dst, src, gain, Rdst):
            n = src.shape[-1]
            c0 = 0
            while c0 < n:
                c1_ = min(n, c0 + 1408)
                b = ctr[0] % 2
                ctr[0] += 1
                fw.dma("sp", st_tiles[b][:, 0:c1_ - c0], src[:, c0:c1_], writes=[R_st[b]])
                fw.op("dve", TS(dst[:, c0:c1_], st_tiles[b][:, 0:c1_ - c0], gain, None, ALU.mult),
                      reads=[R_st[b], R_C], wadd=[Rdst])
                c0 = c1_

        def rstd_act(dst, src_ps, Rsrc, Rdst, n):
            p = dst.shape[0]
            fw.op("act", ACT(dst, src_ps, AF.Ln, scale=1.0 / n, bias=epsb[0:p, 0:1]), reads=[Rsrc, R_C], writes=[Rdst])
            fw.op("act", ACT(dst, dst, AF.Exp, scale=-0.5), reads=[Rdst], writes=[Rdst])

        def rms_rstd(ss_ap, rstd_ap, Rs, n, eng2="pool"):
            fw.op("dve", TS(ss_ap, ss_ap, 1.0 / n, EPS, ALU.mult, ALU.add), reads=[Rs], writes=[Rs])
            p = ss_ap.shape[0]
            f = ss_ap.shape[1]
            fw.op("pool", TT(rstd_ap, ss_ap, mhalf[0:p, 0:f], ALU.pow), reads=[Rs, R_C], writes=[Rs])

        def norm_part(xt, Rx, junk, Rj, stat, Rstat, hb, Rhb):
            fw.op("act", ACT(junk[:], xt[:], AF.Square, accum_out=stat[:, 0:1]), reads=[Rx], writes=[Rj, Rstat])
            rms_rstd(stat[:, 0:1], stat[:, 1:2], Rstat, D)
            fw.op("act", ACT(hb[:], xt[:], AF.Copy, scale=stat[:, 1:2]), reads=[Rx, Rstat], writes=[Rhb])

        def norm_transpose(xt, Rx, junk, Rj, stat, Rstat, hb, Rhb, hT, RhT, col0, first):
            norm_part(xt, Rx, junk, Rj, stat, Rstat, hb, Rhb)
            tr_part(hb, Rhb, hT, RhT, col0, first)

        def tr_part(hb, Rhb, hT, RhT, col0, first):
            for k in range(8):
                fw.op("pe", TR(PSB[:, k * 128:(k + 1) * 128], hb[:, k * 128:(k + 1) * 128], identb[:]),
                      reads=[Rhb, R_C], writes=[R_PSB] if k == 0 else [], wadd=[R_PSB] if k else [])
            kw = dict(writes=[RhT]) if first else dict(wadd=[RhT])
            fw.op("dve", CP(hT[:, :, col0:col0 + 128], PSB[:].rearrange("p (k n) -> p k n", k=8)),
                  reads=[R_PSB], **kw)

        for l in range(L):
            xsrc, Rxsrc = (x_in, R_IN) if l == 0 else (XM, R_XM)
            xdst, Rxdst = (out, R_OUT) if l == L - 1 else (XM, R_XM)

            with ExitStack() as ph:
                winb = sbuf(ph, "winb", [128, 8, WIN], BF16)
                R_W = Res("winb")
                wuqb = sbuf(ph, "wuqb", [128, 2, 768], BF16)
                wkb = sbuf(ph, "wkb", [128, 256], BF16)
                wvb = sbuf(ph, "wvb", [128, 256], BF16)
                gpre = sbuf(ph, "gpre", [128, 8], F32)
                qng = sbuf(ph, "qng", [128, 2], F32)
                kvng = sbuf(ph, "kvng", [128, 1], F32)
                wst = [sbuf(ph, "wst%d" % i, [128, 1408], F32) for i in range(2)]
                R_wst = [Res() for _ in range(2)]
                xin = [sbuf(ph, "xin%d" % i, [128, D], F32) for i in range(2)]
                R_xin = [Res() for _ in range(2)]
                junk = sbuf(ph, "junk", [128, D], BF16)
                R_junk = Res()
                stat = [sbuf(ph, "stat%d" % i, [128, 8], F32) for i in range(2)]
                R_stat = [Res() for _ in range(2)]
                hb = [sbuf(ph, "hb%d" % i, [128, D], BF16) for i in range(2)]
                R_hb = [Res() for _ in range(2)]
                hT = [sbuf(ph, "hT%d" % i, [128, 8, 512], BF16) for i in range(2)]
                R_hT = [Res() for _ in range(2)]
                stg = [sbuf(ph, "stg%d" % i, [128, 512], BF16) for i in range(3)]
                R_stg = [Res() for _ in range(3)]
                kmT = sbuf(ph, "kmT", [128, 3, 2, 16], BF16)
                R_km = Res()
                gsb = sbuf(ph, "gsb", [128, 4, 6, 16], F32)
                R_gsb = Res()
                mx8 = sbuf(ph, "mx8", [128, 4, 6, 8], F32)
                R_mx = Res()
                penf = sbuf(ph, "penf", [128, 4, 6, 16], F32)
                penb = sbuf(ph, "penb", [128, 4, 96], BF16)
                R_pen = Res()
                penT = sbuf(ph, "penT", [128, 512], BF16)
                R_penT = Res()
                cqn = sbuf(ph, "cqn", [128, 2, 512], BF16)
                sqq = sbuf(ph, "sqq", [128, 2, 512], BF16)
                R_cq = Res()
                ckvn = sbuf(ph, "ckvn", [128, 512], BF16)
                sqk = sbuf(ph, "sqk", [128, 512], BF16)
                R_ckv = Res()
                rq = sbuf(ph, "rq", [128, 512], F32)
                R_rq = Res()
                rk = sbuf(ph, "rk", [128, 512], F32)
                R_rk = Res()
                rkt = sbuf(ph, "rkt", [128, 8], F32)
                R_rkt = Res()
                cosb = sbuf(ph, "cosb", [128, 512], F32)
                sinb = sbuf(ph, "sinb", [128, 512], F32)
                R_cs = Res()
                qa = sbuf(ph, "qa", [128, 512], F32)
                qr = sbuf(ph, "qr", [128, 512], F32)
                R_qa = Res()
                vst = [sbuf(ph, "vst%d" % i, [128, 16, 66], BF16) for i in range(2)]
                R_vst = [Res() for _ in range(2)]
                fe = sbuf(ph, "fe", [6, 512], F32)
                fsc = sbuf(ph, "fsc", [6, 512], F32)
                fr = sbuf(ph, "fr", [6, 512], F32)
                fcar = sbuf(ph, "fcar", [6, 1], F32)
                fsp = sbuf(ph, "fsp", [6, 3, 512], BF16)
                fsn = sbuf(ph, "fsn", [6, 3, 512], BF16)
                R_f = Res()
                R_fs = Res()

                fw.dma("sp", gpre[:], g_mixpre[l], writes=[R_C])
                fw.dma("sp", qng[:], qn[l], wadd=[R_C])
                fw.dma("sp", kvng[:], kvn[l], wadd=[R_C])
                ctr = [0]
                for k in range(8):
                    load_scaled_weight(wst, R_wst, ctr, winb[:, k, :], w_in[l, k * 128:(k + 1) * 128, :],
                                       gpre[:, k:k + 1], R_W)
                fw.dma("pool", wuqb[:], w_uq[l].rearrange("(k p) n -> p k n", p=128), wadd=[R_W])
                fw.dma("pool", wkb[:], w_ukvk[l], wadd=[R_W])
                fw.dma("pool", wvb[:], w_ukvv[l], wadd=[R_W])
                fw.op("pool", MS(kmT[:], 0.0), writes=[R_km])
                for b in range(2):
                    fw.op("pool", MS(vst[b][:, :, 64:65], 1.0), writes=[R_vst[b]])
                    fw.op("pool", MS(vst[b][:, :, 65:66], 0.0), wadd=[R_vst[b]])
                fw.op("pool", MS(fcar[:], 0.0), writes=[R_f])

                def load_x(ti):
                    b = ti % 2
                    fw.dma("sp", xin[b][:], xsrc[ti * 128:(ti + 1) * 128, :], reads=[Rxsrc], writes=[R_xin[b]])

                def fchunk(hTg, RhTg, c0, m):
                    p, Rp = getps()
                    for k in range(8):
                        fw.op("pe", MM(p[0:m, :], winb[:, k, c0:c0 + m], hTg[:, k, :], start=(k == 0), stop=(k == 7)),
                              reads=[R_W, RhTg], writes=[Rp] if k == 0 else [], wadd=[Rp] if k else [])
                    return p, Rp

                stgrr = [0]

                def getstg():
                    i = stgrr[0]
                    stgrr[0] = (i + 1) % 3
                    return stg[i], R_stg[i]

                def front_norm(ti):
                    if ti + 1 < NT:
                        load_x(ti + 1)
                    b = ti % 2
                    norm_part(xin[b], R_xin[b], junk, R_junk, stat[b], R_stat[b], hb[b], R_hb[b])

                def front_tr(ti):
                    b = ti % 2
                    gg, i = ti // 4, ti % 4
                    tr_part(hb[b], R_hb[b], hT[gg % 2], R_hT[gg % 2], i * 128, i == 0)

                def proj(g):
                    t0 = g * 512
                    hTg, RhTg = hT[g % 2], R_hT[g % 2]
                    fw.stage("A")
                    fw.dma("sp", cosb[64:96, :], cosd[64:96, t0:t0 + 512], writes=[R_cs])
                    fw.dma("sp", sinb[64:96, :], sind[64:96, t0:t0 + 512], wadd=[R_cs])
                    for c in (3, 4, 5, 9, 10, 11):
                        p, Rp = fchunk(hTg, RhTg, c * 128, 128)
                        s_, Rs_ = getstg()
                        if c < 6:
                            for j in range(2):
                                fw.op("act", ACT(s_[:, j * 256:(j + 1) * 256], p[:, j * 256:(j + 1) * 256], AF.Copy,
                                                 accum_out=qa[:, j:j + 1]), reads=[Rp],
                                      **(dict(writes=[Rs_, R_qa]) if j == 0 else dict(wadd=[Rs_, R_qa])))
                            for hh in range(2):
                                fw.op("act", ACT(kmT[hh * 64:(hh + 1) * 64, c - 3, hh, 2 * g:2 * g + 2], qa[hh * 64:(hh + 1) * 64, 0:2],
                                                 AF.Copy, scale=1.0 / 256), reads=[R_qa], wadd=[R_km])
                        else:
                            fw.op("act", ACT(s_[:], p[:], AF.Copy), reads=[Rp], writes=[Rs_])
                        hd0 = 2 * (c - 3) if c < 6 else 6 + 2 * (c - 9)
                        for hh in range(2):
                            fw.dma("sp", KT[hd0 + hh, 0:64, t0:t0 + 512], s_[hh * 64:(hh + 1) * 64, :],
                                   reads=[Rs_], wadd=[R_KT[hd0 + hh]])
                    yield
                    fw.stage("B")
                    pg, Rpg = getps()
                    for c in (0, 1, 2):
                        p, Rp = fchunk(hTg, RhTg, c * 128, 128)
                        s_, Rs_ = getstg()
                        fw.op("act", ACT(s_[:], p[:], AF.Copy, scale=SCALE_A), reads=[Rp], writes=[Rs_])
                        for hh in range(2):
                            fw.dma("sp", QT[2 * c + hh, 0:64, t0:t0 + 512], s_[hh * 64:(hh + 1) * 64, :],
                                   reads=[Rs_], wadd=[R_QT[2 * c + hh]])
                        for i in range(4):
                            o = (i * 6 + 2 * c) * 16
                            first = (c == 0 and i == 0)
                            fw.op("pe", MM(pg[:, o:o + 32], s_[:, i * 128:(i + 1) * 128],
                                           kmT[:, c, :, :].rearrange("p a n -> p (a n)")),
                                  reads=[Rs_, R_km], writes=[Rpg] if first else [], wadd=[] if first else [Rpg])
                    fw.op("dve", CP(gsb[:].rearrange("p a b c -> p (a b c)"), pg[:, 0:384]), reads=[Rpg], writes=[R_gsb])
                    for i in range(4):
                        own = (g * 4 + i) // 2
                        fw.op("pool", MS(gsb[:, i, :, own:16], -BIG), reads=[R_gsb], wadd=[R_gsb])
                    for i in range(4):
                        for h in range(6):
                            fw.op("dve", lambda e, i=i, h=h: e.max(out=mx8[:, i, h, :], in_=gsb[:, i, h, :]),
                                  reads=[R_gsb], **(dict(writes=[R_mx]) if (i == 0 and h == 0) else dict(wadd=[R_mx])))
                    for i in range(4):
                        for h in range(6):
                            fw.op("dve", TS(penf[:, i, h, :], gsb[:, i, h, :], mx8[:, i, h, 2:3], BIG, ALU.is_ge, ALU.mult),
                                  reads=[R_gsb, R_mx], wadd=[R_pen])
                    fw.op("dve", TS(penb[:].rearrange("p a b -> p (a b)"), penf[:].rearrange("p a b c -> p (a b c)"),
                                    -BIG, None, ALU.add), reads=[R_pen], writes=[R_pen])
                    for i in range(4):
                        own = (g * 4 + i) // 2
                        fw.op("dve", MS(penb[:, i, :].rearrange("p (h n) -> p h n", h=6)[:, :, own:own + 1], 0.0),
                              reads=[R_pen], wadd=[R_pen])
                    for i in range(4):
                        fw.op("pe", TR(PSB[0:96, i * 128:(i + 1) * 128], penb[:, i, :], identb[:]),
                              reads=[R_pen, R_C], writes=[R_PSB] if i == 0 else [], wadd=[R_PSB] if i else [])
                    fw.op("dve", CP(penT[0:96, :], PSB[0:96, 0:512]), reads=[R_PSB], writes=[R_penT])
                    for h in range(6):
                        fw.dma("sp", QT[h, 64:80, t0:t0 + 512], penT[h * 16:(h + 1) * 16, :], reads=[R_penT],
                               wadd=[R_QT[h]])
                    fw.stage("C")
                    for c in (6, 7, 8):
                        p, Rp = fchunk(hTg, RhTg, c * 128, 128)
                        s_, Rs_ = getstg()
                        fw.op("act", ACT(s_[:], p[:], AF.Copy, scale=SCALE_A), reads=[Rp], writes=[Rs_])
                        for hh in range(2):
                            hd = 6 + 2 * (c - 6) + hh
                            fw.dma("sp", QT[hd, 0:64, t0:t0 + 512], s_[hh * 64:(hh + 1) * 64, :], reads=[Rs_],
                                   wadd=[R_QT[hd]])
                    yield
                    fw.stage("D")
                    for cc in range(2):
                        p, Rp = fchunk(hTg, RhTg, (12 + cc) * 128, 128)
                        kw = dict(writes=[R_cq]) if cc == 0 else dict(wadd=[R_cq])
                        fw.op("dve", TS(cqn[:, cc, :], p[:], qng[:, cc:cc + 1], None, ALU.mult), reads=[Rp, R_C], **kw)
                        fw.op("act", ACT(sqq[:, cc, :], p[:], AF.Square), reads=[Rp], wadd=[R_cq])
                    fw.stage("D1")
                    p, Rp = getps()
                    for cc in range(2):
                        fw.op("pe", MM(p[:], onesb[:], sqq[:, cc, :], start=(cc == 0), stop=(cc == 1)),
                              reads=[R_cq, R_C], writes=[Rp] if cc == 0 else [], wadd=[Rp] if cc else [])
                    fw.stage("D1b")
                    rstd_act(rq[:], p[:], Rp, R_rq, 256)
                    fw.stage("D2")
                    for hh in range(4):
                        pa, Rpa = getps()
                        pr, Rpr = getps()
                        for cc in range(2):
                            fw.op("pe", MM(pa[0:96, :], wuqb[:, cc, hh * 192:hh * 192 + 96], cqn[:, cc, :],
                                           start=(cc == 0), stop=(cc == 1)),
                                  reads=[R_W, R_cq], writes=[Rpa] if cc == 0 else [], wadd=[Rpa] if cc else [])
                        for cc in range(2):
                            fw.op("pe", MM(pr[0:96, :], wuqb[:, cc, hh * 192 + 96:hh * 192 + 192], cqn[:, cc, :],
                                           start=(cc == 0), stop=(cc == 1)),
                                  reads=[R_W, R_cq], writes=[Rpr] if cc == 0 else [], wadd=[Rpr] if cc else [])
                        fw.stage("D3")
                        fw.op("dve", STT(qa[0:96, :], pa[0:96, :], SCALE_B, rq[0:96, :], ALU.mult, ALU.mult),
                              reads=[Rpa, R_rq], writes=[R_qa])
                        fw.op("dve", STT(qr[64:96, :], pr[64:96, :], SCALE_B, rq[64:96, :], ALU.mult, ALU.mult),
                              reads=[Rpr, R_rq], wadd=[R_qa])
                        fw.op("dve", TT(qa[64:96, :], qa[64:96, :], cosb[64:96, :], ALU.mult), reads=[R_qa, R_cs], writes=[R_qa])
                        fw.op("dve", TT(qr[64:96, :], qr[64:96, :], sinb[64:96, :], ALU.mult), reads=[R_qa, R_cs], writes=[R_qa])
                        fw.op("dve", TT(qa[64:96, :], qa[64:96, :], qr[64:96, :], ALU.add), reads=[R_qa], writes=[R_qa])
                        s_, Rs_ = getstg()
                        fw.op("act", ACT(s_[0:96, :], qa[0:96, :], AF.Copy), reads=[R_qa], writes=[Rs_])
                        fw.dma("sp", QT[12 + hh, 0:96, t0:t0 + 512], s_[0:96, :], reads=[Rs_], wadd=[R_QT[12 + hh]])
                    fw.stage("E")
                    p, Rp = fchunk(hTg, RhTg, 14 * 128, 128)
                    fw.op("dve", TS(ckvn[:], p[:], kvng[:, 0:1], None, ALU.mult), reads=[Rp, R_C], writes=[R_ckv])
                    fw.op("act", ACT(sqk[:], p[:], AF.Square), reads=[Rp], wadd=[R_ckv])
                    p, Rp = getps()
                    fw.op("pe", MM(p[:], onesb[:], sqk[:]), reads=[R_ckv, R_C], writes=[Rp])
                    rstd_act(rk[:], p[:], Rp, R_rk, 128)
                    p, Rp = getps()
                    for i in range(4):
                        fw.op("pe", MM(p[:, 2 * i:2 * i + 2], sqk[:, i * 128:(i + 1) * 128], onesb[:, 0:2]),
                              reads=[R_ckv, R_C], writes=[Rp] if i == 0 else [], wadd=[Rp] if i else [])
                    rstd_act(rkt[:], p[:, 0:8], Rp, R_rkt, 128)
                    for pair in range(2):
                        p, Rp = getps()
                        fw.op("pe", MM(p[:], wkb[:, pair * 128:(pair + 1) * 128], ckvn[:]), reads=[R_W, R_ckv], writes=[Rp])
                        s_, Rs_ = getstg()
                        fw.op("dve", TT(s_[:], p[:], rk[:], ALU.mult), reads=[Rp, R_rk], writes=[Rs_])
                        for hh in range(2):
                            hd = 12 + 2 * pair + hh
                            fw.dma("sp", KT[hd, 0:64, t0:t0 + 512], s_[hh * 64:(hh + 1) * 64, :], reads=[Rs_],
                                   wadd=[R_KT[hd]])
                    yield
                    fw.stage("F")
                    pk, Rpk = getps()
                    pk2, Rpk2 = getps()
                    for k in range(8):
                        fw.op("pe", MM(pk[0:96, :], winb[:, k, 1920:2016], hTg[:, k, :], start=(k == 0), stop=(k == 7)),
                              reads=[R_W, RhTg], writes=[Rpk] if k == 0 else [], wadd=[Rpk] if k else [])
                    for k in range(8):
                        fw.op("pe", MM(pk2[0:96, :], winb[:, k, 2016:2112], hTg[:, k, :], start=(k == 0), stop=(k == 7)),
                              reads=[R_W, RhTg], writes=[Rpk2] if k == 0 else [], wadd=[Rpk2] if k else [])
                    fw.op("dve", TT(qa[64:96, :], pk[64:96, :], cosb[64:96, :], ALU.mult), reads=[Rpk, R_cs], writes=[R_qa])
                    fw.op("dve", TT(qr[64:96, :], pk2[64:96, :], sinb[64:96, :], ALU.mult), reads=[Rpk2, R_cs], wadd=[R_qa])
                    s_, Rs_ = getstg()
                    fw.op("dve", TT(s_[64:96, :], qa[64:96, :], qr[64:96, :], ALU.add), reads=[R_qa], writes=[Rs_])
                    for hh in range(4):
                        fw.dma("sp", KT[12 + hh, 64:96, t0:t0 + 512], s_[64:96, :], reads=[Rs_], wadd=[R_KT[12 + hh]])
                    pf, Rpf = getps()
                    for k in range(8):
                        fw.op("pe", MM(pf[0:6, :], winb[:, k, 2112:2118], hTg[:, k, :], start=(k == 0), stop=(k == 7)),
                              reads=[R_W, RhTg], writes=[Rpf] if k == 0 else [], wadd=[Rpf] if k else [])
                    fw.op("act", ACT(fe[:], pf[0:6, :], AF.Exp, scale=-1.0, bias=negbf[:, l:l + 1]), reads=[Rpf, R_C], writes=[R_f])
                    fw.op("act", ACT(fe[:], fe[:], AF.Ln, bias=onesf[0:6, 0:1]), reads=[R_f, R_C], writes=[R_f])
                    fw.op("dve", lambda e: e.tensor_tensor_scan(out=fsc[:], data0=onesf[0:6, :], data1=fe[:], initial=fcar[:, 0:1],
                                                                op0=ALU.mult, op1=ALU.add), reads=[R_f, R_C], writes=[R_f])
                    fw.op("dve", CP(fcar[:], fsc[:, 511:512]), reads=[R_f], writes=[R_f])
                    fw.op("dve", CP(fsp[:, 0, :], fsc[:]), reads=[R_f], writes=[R_fs])
                    fw.op("dve", TT(fr[:], fsc[:], fsp[:, 0, :], ALU.subtract), reads=[R_f, R_fs], writes=[R_f])
                    fw.op("dve", CP(fsp[:, 1, :], fr[:]), reads=[R_f], writes=[R_fs])
                    fw.op("dve", TT(fr[:], fr[:], fsp[:, 1, :], ALU.subtract), reads=[R_f, R_fs], writes=[R_f])
                    fw.op("dve", CP(fsp[:, 2, :], fr[:]), reads=[R_f], writes=[R_fs])
                    fw.op("dve", TS(fsn[:].rearrange("p a n -> p (a n)"), fsp[:].rearrange("p a n -> p (a n)"), -1.0, None, ALU.mult),
                          reads=[R_fs], writes=[R_fs])
                    for h in range(6):
                        fw.dma("sp", KT[6 + h:7 + h, 67:70, t0:t0 + 512], fsp[h:h + 1, :, :], reads=[R_fs], wadd=[R_KT[6 + h]])
                        fw.dma("sp", QT[6 + h:7 + h, 64:67, t0:t0 + 512], fsn[h:h + 1, :, :], reads=[R_fs], wadd=[R_QT[6 + h]])
                    fw.stage("G")
                    for i in range(4):
                        ti = g * 4 + i
                        vb, Rvb = vst[ti % 2], R_vst[ti % 2]
                        for (c0, h0) in ((VOFF, 0), (VOFF + 384, 6)):
                            p, Rp = getps()
                            for k in range(8):
                                fw.op("pe", MM(p[:, 0:384], hTg[:, k, i * 128:(i + 1) * 128], winb[:, k, c0:c0 + 384],
                                               start=(k == 0), stop=(k == 7)),
                                      reads=[R_W, RhTg], writes=[Rp] if k == 0 else [], wadd=[Rp] if k else [])
                            kw = dict(writes=[Rvb]) if h0 == 0 else dict(wadd=[Rvb])
                            fw.op("act", ACT(vb[:, h0:h0 + 6, 0:64], p[:, 0:384].rearrange("p (h d) -> p h d", h=6), AF.Copy),
                                  reads=[Rp], **kw)
                        p, Rp = getps()
                        fw.op("pe", MM(p[:, 0:256], ckvn[:, i * 128:(i + 1) * 128], wvb[:]), reads=[R_W, R_ckv], writes=[Rp])
                        fw.op("dve", TS(vb[:, 12:16, 0:64], p[:, 0:256].rearrange("p (h d) -> p h d", h=4),
                                        rkt[:, 2 * i:2 * i + 1], None, ALU.mult), reads=[Rp, R_rkt], wadd=[Rvb])
                        fw.dma("sp", VA[ti * 128:(ti + 1) * 128, :], vb[:].rearrange("p h d -> p (h d)"), reads=[Rvb],
                               wadd=[R_VA])

                load_x(0)
                for i in range(4):
                    front_norm(i)
                    front_tr(i)
                for g in range(NG):
                    gen = proj(g)
                    for i in range(4):
                        ti = (g + 1) * 4 + i
                        if g + 1 < NG:
                            front_norm(ti)
                        next(gen, None)
                        if g + 1 < NG:
                            front_tr(ti)
                    for _ in gen:
                        pass
                fw.flush()

            with ExitStack() as ph:
                Gb = sbuf(ph, "Gb", [128, 6, GW], BF16)
                R_G = Res()
                KTs = [sbuf(ph, "KTs%d" % i, [128, S], BF16) for i in range(2)]
                QTs = [sbuf(ph, "QTs%d" % i, [128, S], BF16) for i in range(2)]
                R_KQ = [Res() for _ in range(2)]
                Vs = [sbuf(ph, "Vs%d" % i, [128, NT * 396 + 128], BF16) for i in range(2)]
                R_Vs = [Res() for _ in range(2)]
                pT = [sbuf(ph, "pT%d" % i, [128, 512], BF16) for i in range(4)]
                R_pT = [Res() for _ in range(4)]
                rden = [sbuf(ph, "rden%d" % i, [128, 512], F32) for i in range(2)]
                bc = [sbuf(ph, "bc%d" % i, [64, 512], F32) for i in range(2)]
                ost = [sbuf(ph, "ost%d" % i, [64, 512], BF16) for i in range(2)]
                R_fin = [Res() for _ in range(2)]
                for h in range(6):
                    fw.dma("pool", Gb[:, h, :], Gd[:, h, :], wadd=[R_G])
                for i in range(2):
                    fw.op("pool", MS(rden[i][:], 0.0), writes=[R_fin[i]])
                    fw.op("pool", MS(Vs[i][:], 0.0), writes=[R_Vs[i]])
                    fw.op("pool", MS(KTs[i][:], 0.0), writes=[R_KQ[i]])
                heads = []
                for h in range(6):
                    heads.append((h, 80, 0, h, h * 64, "A"))
                for h in range(6):
                    heads.append((6 + h, 70, 1, h, 640 + h * 64, "C"))
                for h in range(4):
                    heads.append((12 + h, 96, 2, h, 384 + h * 64, "B"))
                vg_range = {0: (0, 6), 1: (6, 6), 2: (12, 4)}

                def load_head(n):
                    hd, Kc, vg, vi, row0, ty = heads[n]
                    b = n % 2
                    fw.op("pool", MS(QTs[b][64:128, :], 0.0), writes=[R_KQ[b]])
                    fw.dma("sp", QTs[b][0:Kc, :], QT[hd, 0:Kc, :], reads=[R_QT[hd]], writes=[R_KQ[b]])
                    fw.dma("sp", KTs[b][0:Kc, :], KT[hd, 0:Kc, :], reads=[R_KT[hd]], wadd=[R_KQ[b]])

                def load_v(vg):
                    h0, nh = vg_range[vg]
                    b = vg % 2
                    fw.dma("sp", Vs[b][:, 0:NT * 396].rearrange("p (t c) -> p t c", c=396)[:, :, 0:nh * 66],
                           VA[:, h0 * 66:(h0 + nh) * 66].rearrange("(t p) c -> p t c", p=128),
                           reads=[R_VA], writes=[R_Vs[b]])

                tasks = []
                for n, hdinfo in enumerate(heads):
                    for g in range(NG):
                        for kt in range(4 * (g + 1)):
                            tasks.append((n, g, kt))
                load_head(0)
                load_v(0)
                srr = [0]
                state = {}

                def emit_qk(tk):
                    n, g, kt = tk
                    hd, Kc, vg, vi, row0, ty = heads[n]
                    b = n % 2
                    si = srr[0]
                    srr[0] = (si + 1) % 4
                    p, Rp = PS[si], R_PS[si]
                    j = kt - 4 * g
                    c0 = 128 * j if j >= 0 else 0
                    extra = (ty == "A") or (j >= 0)
                    fw.op("pe", MM(p[:, c0:512], KTs[b][:, kt * 128:(kt + 1) * 128], QTs[b][:, g * 512 + c0:(g + 1) * 512],
                                   start=True, stop=not extra), reads=[R_KQ[b]], writes=[Rp])
                    if ty == "A":
                        off = min(512 * g - 128 * kt, 1024) + 384
                        fw.op("pe", MM(p[:, c0:512], identb[:], Gb[:, vi, off + c0:off + 512], start=False, stop=True),
                              reads=[R_G, R_C], wadd=[Rp])
                    elif j >= 0:
                        fw.op("pe", MM(p[:, c0:c0 + 128], identb[:], trib[:], start=False, stop=True), reads=[R_C], wadd=[Rp])
                    if ty != "A" and WARM:
                        fw.op("pe", MM(PS[6][:, :], identb[:], Gb[:, 0, 0:512]), reads=[R_G, R_C], writes=[R_PS[6]])
                    fw.op("act", ACT(pT[si][:, c0:512], p[:, c0:512], AF.Exp), reads=[Rp], writes=[R_pT[si]])
                    state[tk] = (si, c0)

                orr = [0]
                ostate = {}

                def emit_pv(tk):
                    n, g, kt = tk
                    hd, Kc, vg, vi, row0, ty = heads[n]
                    si, c0 = state.pop(tk)
                    if kt == 0:
                        oi = orr[0]
                        orr[0] = (oi + 1) % 2
                        ostate[(n, g)] = oi
                        for ff, age in [x for x in pend if x[0][2] == oi]:
                            emit_fin(ff)
                            emit_fin2(ff)
                        pend[:] = [x for x in pend if x[0][2] != oi]
                        for ff, age in [x for x in pend2 if x[0][2] == oi]:
                            emit_fin2(ff)
                        pend2[:] = [x for x in pend2 if x[0][2] != oi]
                    oi = ostate[(n, g)]
                    po, Rpo = PS[4 + oi], R_PS[4 + oi]
                    last = (kt == 4 * (g + 1) - 1)
                    vb0 = kt * 396 + vi * 66
                    fw.op("pe", MM(po[:, c0:512], Vs[vg % 2][:, vb0:vb0 + 128], pT[si][:, c0:512],
                                   start=(kt == 0), stop=last),
                          reads=[R_Vs[vg % 2], R_pT[si]], writes=[Rpo] if kt == 0 else [], wadd=[Rpo] if kt else [])
                    if last:
                        fw.op("dve", lambda e, oi=oi, po=po: e.reciprocal(out=rden[oi][64:65, :], in_=po[64:65, :]),
                              reads=[Rpo], writes=[R_fin[oi]])
                        return (n, g, oi)
                    return None

                def emit_fin(f):
                    n, g, oi = f
                    pb, Rpb = PS[6], R_PS[6]
                    fw.op("pe", MM(pb[0:64, :], selrow[:], rden[oi][:]), reads=[R_fin[oi], R_C], writes=[Rpb])

                def emit_fin2(f):
                    n, g, oi = f
                    hd, Kc, vg, vi, row0, ty = heads[n]
                    po, Rpo = PS[4 + oi], R_PS[4 + oi]
                    pb, Rpb = PS[6], R_PS[6]
                    fw.op("dve", CP(bc[oi][:], pb[0:64, :]), reads=[Rpb], wadd=[R_fin[oi]])
                    fw.op("dve", TT(ost[oi][:], po[0:64, :], bc[oi][:], ALU.mult), reads=[Rpo, R_fin[oi]], wadd=[R_fin[oi]])
                    fw.dma("sp", MT[row0:row0 + 64, g * 512:(g + 1) * 512], ost[oi][:], reads=[R_fin[oi]], wadd=[R_MT])

                pend = []
                pend2 = []
                LOOK = 2
                for i0 in range(min(LOOK, len(tasks))):
                    emit_qk(tasks[i0])
                for idx, tk in enumerate(tasks):
                    n, g, kt = tk
                    if g == 0 and kt == 0:
                        if n + 1 < len(heads):
                            load_head(n + 1)
                            if heads[n + 1][2] != heads[n][2]:
                                load_v(heads[n + 1][2])
                    if idx + LOOK < len(tasks):
                        emit_qk(tasks[idx + LOOK])
                    f = emit_pv(tk)
                    pend[:] = [(ff, age + 1) for (ff, age) in pend]
                    pend2[:] = [(ff, age + 1) for (ff, age) in pend2]
                    while pend2 and pend2[0][1] >= 2:
                        emit_fin2(pend2.pop(0)[0])
                    while pend and pend[0][1] >= 5:
                        ff = pend.pop(0)[0]
                        emit_fin(ff)
                        pend2.append((ff, 0))
                    if f is not None:
                        pend.append((f, 0))
                for ff, age in pend2:
                    emit_fin2(ff)
                for ff, age in pend:
                    emit_fin(ff)
                    emit_fin2(ff)
                fw.flush()

            with ExitStack() as ph:
                wob = sbuf(ph, "wob", [128, 8, D], BF16)
                R_W = Res()
                gpost = sbuf(ph, "gpost", [128, D], F32)
                mT = [sbuf(ph, "mT%d" % i, [128, 8, 512], BF16) for i in range(2)]
                R_mT = [Res() for _ in range(2)]
                xin = [sbuf(ph, "xin%d" % i, [128, D], F32) for i in range(2)]
                R_xin = [Res() for _ in range(2)]
                junk = sbuf(ph, "junk", [128, 512], BF16)
                R_junk = Res()
                stat = [sbuf(ph, "stat%d" % i, [128, 8], F32) for i in range(2)]
                R_stat = [Res() for _ in range(2)]
                yo = [sbuf(ph, "yo%d" % i, [128, D], F32) for i in range(2)]
                R_yo = [Res() for _ in range(2)]
                fw.dma("pool", wob[:], w_o[l].rearrange("(k p) n -> p k n", p=128), writes=[R_W])
                fw.dma("sp", gpost[:], g_mixpost[l], wadd=[R_W])

                def load_m(g):
                    fw.dma("sp", mT[g % 2][:], MT[:, g * 512:(g + 1) * 512].rearrange("(k p) n -> p k n", p=128),
                           reads=[R_MT], writes=[R_mT[g % 2]])
                load_m(0)
                for g in range(NG):
                    if g + 1 < NG:
                        load_m(g + 1)
                    for i in range(4):
                        ti = g * 4 + i
                        b = ti % 2
                        fw.dma("sp", xin[b][:], xsrc[ti * 128:(ti + 1) * 128, :], reads=[Rxsrc], writes=[R_xin[b]])
                        pp = []
                        for half in range(2):
                            p, Rp = getps()
                            pp.append((p, Rp))
                            for k in range(8):
                                fw.op("pe", MM(p[:], mT[g % 2][:, k, i * 128:(i + 1) * 128], wob[:, k, half * 512:(half + 1) * 512],
                                               start=(k == 0), stop=(k == 7)),
                                      reads=[R_W, R_mT[g % 2]], writes=[Rp] if k == 0 else [], wadd=[Rp] if k else [])
                        for half in range(2):
                            p, Rp = pp[half]
                            fw.op("act", ACT(junk[:], p[:], AF.Square, accum_out=stat[b][:, half:half + 1]),
                                  reads=[Rp], writes=[R_junk], wadd=[R_stat[b]])
                        fw.op("dve", TT(stat[b][:, 2:3], stat[b][:, 0:1], stat[b][:, 1:2], ALU.add), reads=[R_stat[b], R_junk],
                              writes=[R_stat[b]])
                        rms_rstd(stat[b][:, 2:3], stat[b][:, 3:4], R_stat[b], D)
                        for half in range(2):
                            p, Rp = pp[half]
                            sl = slice(half * 512, (half + 1) * 512)
                            fw.op("dve", STT(yo[b][:, sl], p[:], stat[b][:, 3:4], gpost[:, sl], ALU.mult, ALU.mult),
                                  reads=[Rp, R_stat[b], R_W], **(dict(writes=[R_yo[b]]) if half == 0 else dict(wadd=[R_yo[b]])))
                        fw.op("dve", TT(yo[b][:], yo[b][:], xin[b][:], ALU.add), reads=[R_yo[b], R_xin[b]], writes=[R_yo[b]])
                        fw.dma("sp", X1[ti * 128:(ti + 1) * 128, :], yo[b][:], reads=[R_yo[b]], wadd=[R_X1])
                fw.flush()

            with ExitStack() as ph:
                wupb = sbuf(ph, "wupb", [128, 8, 2 * DFF], BF16)
                wdnb = sbuf(ph, "wdnb", [128, 22, D], BF16)
                R_W = Res()
                gpre = sbuf(ph, "gpre", [128, 8], F32)
                gpost = sbuf(ph, "gpost", [128, D], F32)
                cvp = sbuf(ph, "cvp", [128, NCH_UP, 4], F32)
                fw.dma("sp", gpre[:], g_ffnpre[l], writes=[R_C])
                fw.dma("sp", gpost[:], g_ffnpost[l], wadd=[R_C])
                fw.dma("sp", cvp[:], convp[l], wadd=[R_C])
                with ExitStack() as ph2:
                    wst = [sbuf(ph2, "wst%d" % i, [128, 1408], F32) for i in range(2)]
                    R_wst = [Res() for _ in range(2)]
                    ctr = [0]
                    for k in range(8):
                        load_scaled_weight(wst, R_wst, ctr, wupb[:, k, :], w_up[l, k * 128:(k + 1) * 128, :], gpre[:, k:k + 1], R_W)
                    fw.dma("pool", wdnb[:, 0:11, :], w_down[l, 0:1408, :].rearrange("(c p) n -> p c n", p=128), wadd=[R_W])
                    fw.dma("pool", wdnb[:, 11:22, :], w_down[l, 1408:2816, :].rearrange("(c p) n -> p c n", p=128), wadd=[R_W])
                    fw.flush()
                xin = [sbuf(ph, "xin%d" % i, [128, D], F32) for i in range(4)]
                R_xin = [Res() for _ in range(4)]
                junk = sbuf(ph, "junk", [128, D], BF16)
                R_junk = Res()
                stat = [sbuf(ph, "stat%d" % i, [128, 8], F32) for i in range(2)]
                R_stat = [Res() for _ in range(2)]
                hb = [sbuf(ph, "hb%d" % i, [128, D], BF16) for i in range(2)]
                R_hb = [Res() for _ in range(2)]
                hT = [sbuf(ph, "hT%d" % i, [128, 8, 256], BF16) for i in range(2)]
                R_hT = [Res() for _ in range(2)]
                aT = sbuf(ph, "aT", [128, 22, 256], BF16)
                R_aT = Res()
                NUE = 4
                uext = [sbuf(ph, "uext%d" % i, [128, 258], F32) for i in range(NUE)]
                R_ue = [Res() for _ in range(NUE)]
                acc = [sbuf(ph, "acc%d" % i, [128, 256], F32) for i in range(NUE)]
                R_acc = [Res() for _ in range(NUE)]
                gl = [sbuf(ph, "gl%d" % i, [128, 256], F32) for i in range(2)]
                R_gl = [Res() for _ in range(2)]
                halo = sbuf(ph, "halo", [128, NCH_UP, 2], F32)
                R_halo = [Res() for _ in range(NCH_UP)]
                yo = [sbuf(ph, "yo%d" % i, [128, D], F32) for i in range(2)]
                R_yo = [Res() for _ in range(2)]
                fw.op("pool", MS(halo[:], 0.0), writes=R_halo)
                NG2 = S // 256

                def load_x4(ti):
                    b = ti % 4
                    fw.dma("sp", xin[b][:], X1[ti * 128:(ti + 1) * 128, :], reads=[R_X1], writes=[R_xin[b]])
                load_x4(0)
                load_x4(1)
                for g in range(NG2):
                    hTg, RhTg = hT[g % 2], R_hT[g % 2]
                    for i in range(2):
                        ti = g * 2 + i
                        if ti + 2 < NT:
                            load_x4(ti + 2)
                        norm_transpose(xin[ti % 4], R_xin[ti % 4], junk, R_junk, stat[ti % 2], R_stat[ti % 2], hb[ti % 2], R_hb[ti % 2],
                                       hTg, RhTg, i * 128, i == 0)
                    for c in range(22):
                        res = []
                        for which in range(2):
                            ch = c + 22 * which
                            p, Rp = getps()
                            for k in range(8):
                                fw.op("pe", MM(p[:, 0:256], wupb[:, k, ch * 128:(ch + 1) * 128], hTg[:, k, :], start=(k == 0), stop=(k == 7)),
                                      reads=[R_W, RhTg], writes=[Rp] if k == 0 else [], wadd=[Rp] if k else [])
                            bi = (2 * c + which) % NUE
                            ue, Rue = uext[bi], R_ue[bi]
                            ac, Rac = acc[bi], R_acc[bi]
                            fw.op("act", ACT(ue[:, 2:258], p[:, 0:256], AF.Copy), reads=[Rp], writes=[Rue])
                            fw.op("pool", CP(ue[:, 0:2], halo[:, ch, :]), reads=[R_halo[ch]], wadd=[Rue])
                            fw.op("act", ACT(ac[:], p[:, 0:256], AF.Identity, scale=cvp[:, ch, 2:3], bias=cvp[:, ch, 3:4]),
                                  reads=[Rp, R_C], writes=[Rac])
                            fw.op("pool", CP(halo[:, ch, :], ue[:, 256:258]), reads=[Rue], writes=[R_halo[ch]])
                            fw.op("dve", STT(ac[:], ue[:, 1:257], cvp[:, ch, 1:2], ac[:], ALU.mult, ALU.add), reads=[Rue, Rac, R_C], writes=[Rac])
                            fw.op("dve", STT(ac[:], ue[:, 0:256], cvp[:, ch, 0:1], ac[:], ALU.mult, ALU.add), reads=[Rue, Rac, R_C], writes=[Rac])
                        ag, av = (2 * c) % NUE, (2 * c + 1) % NUE
                        fw.op("act", ACT(gl[c % 2][:], acc[ag][:], AF.Gelu_apprx_tanh), reads=[R_acc[ag]], writes=[R_gl[c % 2]])
                        fw.op("dve", TT(aT[:, c, :], gl[c % 2][:], acc[av][:], ALU.mult), reads=[R_gl[c % 2], R_acc[av]],
                              **(dict(writes=[R_aT]) if c == 0 else dict(wadd=[R_aT])))
                    for i in range(2):
                        ti = g * 2 + i
                        b = ti % 2
                        xb, Rxb = xin[ti % 4], R_xin[ti % 4]
                        pp = []
                        for half in range(2):
                            p, Rp = getps()
                            pp.append((p, Rp))
                            for c in range(22):
                                fw.op("pe", MM(p[:], aT[:, c, i * 128:(i + 1) * 128], wdnb[:, c, half * 512:(half + 1) * 512],
                                               start=(c == 0), stop=(c == 21)),
                                      reads=[R_W, R_aT], writes=[Rp] if c == 0 else [], wadd=[Rp] if c else [])
                        for half in range(2):
                            p, Rp = pp[half]
                            fw.op("act", ACT(junk[:, 0:512], p[:], AF.Square, accum_out=stat[b][:, 4 + half:5 + half]),
                                  reads=[Rp], writes=[R_junk], wadd=[R_stat[b]])
                        fw.op("dve", TT(stat[b][:, 6:7], stat[b][:, 4:5], stat[b][:, 5:6], ALU.add), reads=[R_stat[b], R_junk],
                              writes=[R_stat[b]])
                        rms_rstd(stat[b][:, 6:7], stat[b][:, 7:8], R_stat[b], D)
                        for half in range(2):
                            p, Rp = pp[half]
                            sl = slice(half * 512, (half + 1) * 512)
                            fw.op("dve", STT(yo[b][:, sl], p[:], stat[b][:, 7:8], gpost[:, sl], ALU.mult, ALU.mult),
                                  reads=[Rp, R_stat[b], R_C], **(dict(writes=[R_yo[b]]) if half == 0 else dict(wadd=[R_yo[b]])))
                        fw.op("pool", TT(yo[b][:], yo[b][:], xb[:], ALU.add), reads=[R_yo[b], Rxb], writes=[R_yo[b]])
                        fw.dma("sp", xdst[ti * 128:(ti + 1) * 128, :], yo[b][:], reads=[R_yo[b]], wadd=[Rxdst])
                fw.flush()
    return nc


def _t5_bucket_np(n):
    n = np.maximum(n, 0)
    exact = 16
    large = exact + (np.log(np.maximum(n, 1).astype(np.float32) / exact) / math.log(1024 / exact) * (32 - exact)).astype(np.int32)
    return np.where(n < exact, n, np.minimum(large, 31))


def host_consts(S):
    c = np.arange(128)[:, None]
    m = np.arange(GW)[None, :]
    r = m - 384 - c
    bidx = _t5_bucket_np(r).astype(np.int64)
    causal = r >= 0
    tri = np.where(np.arange(128)[None, :] >= np.arange(128)[:, None], 0.0, -BIG).astype(np.float32)
    half = 16
    inv = (10000.0 ** (-np.arange(half, dtype=np.float32) / half)).astype(np.float32)
    ang = np.arange(S, dtype=np.float32)[None, :] * inv[:, None]
    cos, sin = np.cos(ang).astype(np.float32), np.sin(ang).astype(np.float32)
    cosT = np.zeros((128, S), np.float32)
    sinS = np.zeros((128, S), np.float32)
    cosT[64:80] = cos
    cosT[80:96] = cos
    sinS[64:80] = -sin
    sinS[80:96] = sin
    blk = (np.arange(S)[None, :] // 256 == np.arange(16)[:, None]).astype(np.float32)
    return bidx, causal, tri, cosT, sinS, blk


def host_layout(inp, S, L):
    bidx, causal, tri, cosT, sinS, blk = host_consts(S)
    rb = np.asarray(inp["rel_bias"], np.float32)
    G = np.where(causal[:, None, :], rb[bidx].transpose(0, 2, 1), np.float32(-BIG)).astype(np.float32)
    w_in = np.asarray(inp["w_in"], np.float32)
    o = {"a_q": 0, "a_k": 384, "a_v": 768, "c_q": 1152, "c_kv": 1408, "k_r": 1536, "f_q": 1568, "f_k": 1952, "f_v": 2336, "f_g": 2720}
    cols = np.concatenate([
        np.arange(o["a_q"], o["a_q"] + 384), np.arange(o["a_k"], o["a_k"] + 384),
        np.arange(o["f_q"], o["f_q"] + 384), np.arange(o["f_k"], o["f_k"] + 384),
        np.arange(o["c_q"], o["c_q"] + 256), np.arange(o["c_kv"], o["c_kv"] + 128),
        np.full(64, -1), np.arange(o["k_r"], o["k_r"] + 32),
        np.full(64, -1), np.arange(o["k_r"] + 16, o["k_r"] + 32), np.arange(o["k_r"], o["k_r"] + 16),
        np.arange(o["f_g"], o["f_g"] + 6), np.full(2, -1),
        np.arange(o["a_v"], o["a_v"] + 384), np.arange(o["f_v"], o["f_v"] + 384)])
    assert cols.shape[0] == WIN
    w_in_r = np.zeros((L, D, WIN), np.float32)
    w_in_r[:, :, cols >= 0] = w_in[:L][:, :, cols[cols >= 0]]
    w_uq = np.asarray(inp["w_uq"], np.float32)[:L]
    uq = w_uq.reshape(L, 256, 4, 96)
    w_uq_r = np.ascontiguousarray(np.concatenate([uq, np.zeros((L, 256, 4, 64), np.float32), uq[..., 80:96], uq[..., 64:80]],
                                                 axis=-1).reshape(L, 256, 768))
    ukv = np.asarray(inp["w_ukv"], np.float32)[:L].reshape(L, 128, 4, 128)
    w_ukvk = np.ascontiguousarray(ukv[..., :64].reshape(L, 128, 256))
    w_ukvv = np.ascontiguousarray(ukv[..., 64:].reshape(L, 128, 256))

    def pk(v):
        return np.ascontiguousarray(np.asarray(v, np.float32)[:L].reshape(L, 8, 128).transpose(0, 2, 1))

    def bc(v):
        return np.ascontiguousarray(np.broadcast_to(np.asarray(v, np.float32)[:L, None, :], (L, 128, D)))
    cw = np.asarray(inp["conv_w"], np.float32)[:L]
    cb = np.asarray(inp["conv_b"], np.float32)[:L]
    cv = np.concatenate([cw, cb[:, None, :]], axis=1)
    convp = np.ascontiguousarray(cv.reshape(L, 4, NCH_UP, 128).transpose(0, 3, 2, 1))
    return {
        "w_in": w_in_r,
        "g_mixpre": pk(inp["ln_mix_pre"]), "g_ffnpre": pk(inp["ln_ffn_pre"]),
        "g_mixpost": bc(inp["ln_mix_post"]), "g_ffnpost": bc(inp["ln_ffn_post"]),
        "b_f": np.ascontiguousarray(np.asarray(inp["b_f"], np.float32)[:L].T),
        "qn": np.ascontiguousarray(np.asarray(inp["q_norm"], np.float32)[:L].reshape(L, 2, 128).transpose(0, 2, 1)),
        "kvn": np.ascontiguousarray(np.asarray(inp["kv_norm"], np.float32)[:L].reshape(L, 128, 1)),
        "w_uq": w_uq_r, "w_ukvk": w_ukvk, "w_ukvv": w_ukvv,
        "w_o": np.ascontiguousarray(np.asarray(inp["w_o"], np.float32)[:L]),
        "w_up": np.ascontiguousarray(np.asarray(inp["w_up"], np.float32)[:L]),
        "convp": convp,
        "w_down": np.ascontiguousarray(np.asarray(inp["w_down"], np.float32)[:L]),
        "G": np.ascontiguousarray(G), "tri": tri, "ident": np.eye(128, dtype=np.float32),
        "cosT": cosT, "sinS": sinS, "blk1h": blk,
    }


_NC_CACHE = {}


def kernel(**inputs):
    x = np.asarray(inputs["x"], np.float32)
    B, S, _ = x.shape
    L = np.asarray(inputs["w_in"]).shape[0]
    shared = host_layout(inputs, S, L)
    key = (S, L)
    if key not in _NC_CACHE:
        _NC_CACHE[key] = build(S, L)
    nc = _NC_CACHE[key]
    in_maps = []
    for b in range(B):
        m = dict(shared)
        m["x"] = np.ascontiguousarray(x[b])
        in_maps.append(m)
    res = run_bass_kernel_spmd(nc, in_maps, core_ids=list(range(B)))
    return np.stack([np.asarray(r["out"], np.float32) for r in res.results], axis=0)
```
